# Optimizing a Trainium2 kernel written in Bass

```python
import math
import jax, jax.numpy as jnp
from jax import lax
import numpy as np

D_MODEL = 1024
BATCH = 4
SEQ = 4096
DEPTH = 1

N_MEM = 256
EPS = 1e-6
GLA_HEADS = 4
GLA_DK = D_MODEL // (2 * GLA_HEADS)
GLA_DV = D_MODEL // GLA_HEADS
GLA_RANK = 16
GLA_GATE_NORM = 16.0
GLA_CHUNK = 64
DSA_HEADS = 8
DSA_KV_HEADS = 2
DSA_HD = D_MODEL // DSA_HEADS
IDX_HEADS = 8
IDX_DIM = 64
TOPK_MAX = 256
Q_BLOCK = 128
REL_BUCKETS = 32
REL_MAX_DIST = 128
X_HEADS = 4
X_HD = D_MODEL // X_HEADS
N_BRANCH = 3

SPLIT_SIZES = (
    GLA_HEADS * GLA_DK,
    GLA_HEADS * GLA_DK,
    GLA_HEADS * GLA_DV,
    GLA_RANK,
    GLA_HEADS * GLA_DV,
    DSA_HEADS * DSA_HD,
    DSA_KV_HEADS * DSA_HD,
    DSA_KV_HEADS * DSA_HD,
    IDX_HEADS * IDX_DIM,
    IDX_DIM,
    IDX_HEADS,
    DSA_HEADS * DSA_HD,
    X_HEADS * X_HD,
    X_HEADS * X_HD,
    N_BRANCH * D_MODEL,
)
W_IN_COLS = 11352

kernel_name = "hybrid_gla_dsa_memxattn_gated_merge"


def rmsnorm(x, g):
    xf = x.astype(jnp.float32)
    y = xf * lax.rsqrt(jnp.mean(xf * xf, axis=-1, keepdims=True) + EPS)
    return (y * g.astype(jnp.float32)).astype(x.dtype)


def split_cols(u):
    offsets = np.cumsum(np.array(SPLIT_SIZES))[:-1].tolist()
    return jnp.split(u, offsets, axis=-1)


def t5_bucket(dist):
    max_exact = REL_BUCKETS // 2
    d = jnp.maximum(dist, 1).astype(jnp.float32)
    large = max_exact + (jnp.log(d / max_exact) / math.log(REL_MAX_DIST / max_exact)
                         * (REL_BUCKETS - max_exact)).astype(jnp.int32)
    large = jnp.minimum(large, REL_BUCKETS - 1)
    return jnp.where(dist < max_exact, dist, large)


def gla_mixer(q, k, v, a_low, z, w_a_up, b_a, g_head):
    B, L, _ = q.shape
    H, dk, dv, C = GLA_HEADS, GLA_DK, GLA_DV, GLA_CHUNK
    nc = L // C
    q = q.reshape(B, nc, C, H, dk) * (dk ** -0.5)
    k = k.reshape(B, nc, C, H, dk)
    v = v.reshape(B, nc, C, H, dv)
    log_a = jax.nn.log_sigmoid((a_low @ w_a_up + b_a).astype(jnp.float32)) / GLA_GATE_NORM
    b = jnp.cumsum(log_a.reshape(B, nc, C, H, dk), axis=2)
    b_last = b[:, :, -1:]
    q_d = q * jnp.exp(b).astype(q.dtype)
    k_d = k * jnp.exp(-b).astype(k.dtype)
    k_t = k * jnp.exp(b_last - b).astype(k.dtype)
    decay = jnp.exp(b_last[:, :, 0]).astype(q.dtype)
    causal = jnp.tril(jnp.ones((C, C), dtype=bool))
    att = jnp.einsum('bnthd,bnshd->bnhts', q_d, k_d)
    att = jnp.where(causal, att, jnp.zeros_like(att))
    o_intra = jnp.einsum('bnhts,bnshv->bnthv', att, v)

    def step(state, inp):
        qc, kc, vc, dc = inp
        o = jnp.einsum('bthd,bhdv->bthv', qc, state)
        state = dc[..., None] * state + jnp.einsum('bthd,bthv->bhdv', kc, vc)
        return state, o

    s0 = jnp.zeros((B, H, dk, dv), q.dtype)
    xs = (q_d.transpose(1, 0, 2, 3, 4), k_t.transpose(1, 0, 2, 3, 4),
          v.transpose(1, 0, 2, 3, 4), decay.transpose(1, 0, 2, 3))
    _, o_inter = lax.scan(step, s0, xs)
    o = o_intra + o_inter.transpose(1, 0, 2, 3, 4)
    o = rmsnorm(o.reshape(B, L, H, dv), g_head)
    return o.reshape(B, L, H * dv) * jax.nn.silu(z)


def dsa_mixer(q, k, v, iq, ik, iw, z, rel_bias):
    B, L, _ = q.shape
    topk = min(TOPK_MAX, L // 4)
    nb = L // Q_BLOCK
    KV = DSA_KV_HEADS
    G = DSA_HEADS // KV
    q = q.reshape(B, nb, Q_BLOCK, KV, G, DSA_HD) * (DSA_HD ** -0.5)
    k = k.reshape(B, L, KV, DSA_HD)
    v = v.reshape(B, L, KV, DSA_HD)
    iq = iq.reshape(B, nb, Q_BLOCK, IDX_HEADS, IDX_DIM)
    iw = iw.reshape(B, nb, Q_BLOCK, IDX_HEADS) * (IDX_HEADS ** -0.5) * (IDX_DIM ** -0.5)
    ik_f = ik.astype(jnp.float32)
    key_pos = jnp.arange(L, dtype=jnp.int32)

    def block(inp):
        qb, iqb, iwb, start = inp
        q_pos = start + jnp.arange(Q_BLOCK, dtype=jnp.int32)
        s = jax.nn.relu(jnp.einsum('bqhd,bsd->bqhs', iqb.astype(jnp.float32), ik_f))
        score = jnp.einsum('bqhs,bqh->bqs', s, iwb.astype(jnp.float32))
        visible = key_pos[None, :] <= q_pos[:, None]
        score = jnp.where(visible[None], score, -jnp.inf)
        _, idx = lax.top_k(score, topk)
        valid = idx <= q_pos[None, :, None]
        k_sel = jax.vmap(lambda kb, ib: kb[ib])(k, idx)
        v_sel = jax.vmap(lambda vb, ib: vb[ib])(v, idx)
        logits = jnp.einsum('bqcgd,bqncd->bqcgn', qb, k_sel).astype(jnp.float32)
        bucket = t5_bucket(jnp.maximum(q_pos[None, :, None] - idx, 0))
        bias = rel_bias[bucket].reshape(B, Q_BLOCK, topk, KV, G).transpose(0, 1, 3, 4, 2)
        logits = logits + bias.astype(jnp.float32)
        logits = jnp.where(valid[:, :, None, None, :], logits, -1e30)
        p = jax.nn.softmax(logits, axis=-1).astype(v.dtype)
        o = jnp.einsum('bqcgn,bqncd->bqcgd', p, v_sel)
        return o.reshape(B, Q_BLOCK, DSA_HEADS * DSA_HD)

    starts = jnp.arange(nb, dtype=jnp.int32) * Q_BLOCK
    xs = (q.transpose(1, 0, 2, 3, 4, 5), iq.transpose(1, 0, 2, 3, 4),
          iw.transpose(1, 0, 2, 3), starts)
    o = lax.map(block, xs)
    o = o.transpose(1, 0, 2, 3).reshape(B, L, DSA_HEADS * DSA_HD)
    return o * jax.nn.silu(z)


def cross_mixer(q, z, mem_n, w_mem_kv):
    B, L, _ = q.shape
    M = mem_n.shape[1]
    mk, mv = jnp.split(mem_n @ w_mem_kv, 2, axis=-1)
    mk = mk.reshape(B, M, X_HEADS, X_HD)
    mv = mv.reshape(B, M, X_HEADS, X_HD)
    q = q.reshape(B, L, X_HEADS, X_HD) * (X_HD ** -0.5)
    logits = jnp.einsum('bthd,bmhd->bhtm', q, mk).astype(jnp.float32)
    p = jax.nn.softmax(logits, axis=-1).astype(mv.dtype)
    o = jnp.einsum('bhtm,bmhd->bthd', p, mv).reshape(B, L, X_HEADS * X_HD)
    return o * jax.nn.silu(z)


def setup_inputs(seed: int = 0) -> dict:
    key = jax.random.key(seed)
    ks = jax.random.split(key, 16)
    D = D_MODEL

    def w(k, shape, fan_in):
        return jax.random.normal(k, shape, jnp.float32) * (fan_in ** -0.5)

    def gain(k, shape):
        return 1.0 + 0.05 * jax.random.normal(k, shape, jnp.float32)

    return {
        "x": jax.random.normal(ks[0], (BATCH, SEQ, D), jnp.float32),
        "mem": jax.random.normal(ks[1], (BATCH, N_MEM, D), jnp.float32),
        "g_pre": gain(ks[2], (DEPTH, D)),
        "g_post": gain(ks[3], (DEPTH, D)),
        "g_mem": gain(ks[4], (DEPTH, D)),
        "w_in": w(ks[5], (DEPTH, D, W_IN_COLS), D),
        "w_gla_a_up": w(ks[6], (DEPTH, GLA_RANK, GLA_HEADS * GLA_DK), GLA_RANK),
        "b_gla_a": 0.1 * jax.random.normal(ks[7], (DEPTH, GLA_HEADS * GLA_DK), jnp.float32),
        "g_gla": gain(ks[8], (DEPTH, GLA_DV)),
        "rel_bias": 0.5 * jax.random.normal(ks[9], (REL_BUCKETS, DSA_HEADS), jnp.float32),
        "w_mem_kv": w(ks[10], (DEPTH, D, 2 * X_HEADS * X_HD), D),
        "w_gla_out": w(ks[11], (DEPTH, GLA_HEADS * GLA_DV, D), GLA_HEADS * GLA_DV),
        "w_dsa_out": w(ks[12], (DEPTH, DSA_HEADS * DSA_HD, D), DSA_HEADS * DSA_HD),
        "w_x_out": w(ks[13], (DEPTH, X_HEADS * X_HD, D), X_HEADS * X_HD),
        "w_o": w(ks[14], (DEPTH, D, D), D),
    }


def reference(x, mem, g_pre, g_post, g_mem, w_in, w_gla_a_up, b_gla_a, g_gla,
              rel_bias, w_mem_kv, w_gla_out, w_dsa_out, w_x_out, w_o):
    for i in range(DEPTH):
        h = rmsnorm(x, g_pre[i])
        (gq, gk, gv, ga, gz, dq, dk, dv, iq, ik, iw, dz, xq, xz, gates) = split_cols(h @ w_in[i])
        y_gla = gla_mixer(gq, gk, gv, ga, gz, w_gla_a_up[i], b_gla_a[i], g_gla[i])
        y_dsa = dsa_mixer(dq, dk, dv, iq, ik, iw, dz, rel_bias)
        y_mem = cross_mixer(xq, xz, rmsnorm(mem, g_mem[i]), w_mem_kv[i])
        s_gla, s_dsa, s_mem = jnp.split(jax.nn.sigmoid(gates), N_BRANCH, axis=-1)
        merged = (s_gla * (y_gla @ w_gla_out[i]) + s_dsa * (y_dsa @ w_dsa_out[i])
                  + s_mem * (y_mem @ w_x_out[i]))
        x = x + rmsnorm(merged @ w_o[i], g_post[i])
    return x
```

```python
from contextlib import ExitStack
import os
import numpy as np
_os_environ_get = os.environ.get
import ml_dtypes
import concourse.bass as bass
import concourse.mybir as mybir
from concourse.bass_utils import run_bass_kernel_spmd

F32 = mybir.dt.float32
BF16 = mybir.dt.bfloat16
AF = mybir.ActivationFunctionType
ALU = mybir.AluOpType
AX = mybir.AxisListType

D = 1024
NOWN = 2048
NCTX = 2048
NTOK = 4096
W_IN_COLS = 11352
EPS = 1e-6
NEG = -1.0e30

_sizes = [512, 512, 1024, 16, 1024, 1024, 256, 256, 512, 64, 8, 1024, 1024, 1024, 3072]
_names = ["gq", "gk", "gv", "ga", "gz", "dq", "dk", "dv", "iq", "ik", "iw", "dz", "xq", "xz", "gates"]
COL = {}
_o = 0
for _n, _s in zip(_names, _sizes):
    COL[_n] = _o
    _o += _s
assert _o == W_IN_COLS

ENGS = ("pe", "act", "dve", "pool", "sp")


class Sch:
    def __init__(self):
        self.ops = {e: [] for e in ENGS}
        self.count = {}
        self.lastw = {}
        self.readers = {}
        self.known = {e: {} for e in ENGS}
        self.dma_sems = set()
        self.floor = {}

    def barrier(self):
        snap = dict(self.count)
        for e in ENGS:
            self.floor[e] = dict(snap)

    def op(self, eng, fn, reads=(), writes=(), dma=None):
        ps_r = [k for k in reads if k == "ptr" or (isinstance(k, tuple) and k[0] == "pb")]
        if ps_r:
            reads = [k for k in reads if k not in ps_r]
            writes = list(writes) + ps_r
        deps = {}

        def need(ev, raw):
            sem, val, src = ev
            if src is not None and src == eng and eng == "pe":
                return
            if deps.get(sem, 0) < val:
                deps[sem] = val

        for k in reads:
            w = self.lastw.get(k)
            if w is not None:
                need(w, True)
        for k in writes:
            w = self.lastw.get(k)
            if w is not None:
                need(w, False)
            for r in self.readers.get(k, {}).values():
                need(r, False)
        fl = self.floor.get(eng)
        if fl:
            for sem, val in fl.items():
                if sem != eng and deps.get(sem, 0) < val:
                    deps[sem] = val
            self.floor[eng] = None
        waits = []
        kn = self.known[eng]
        for sem, val in deps.items():
            if kn.get(sem, 0) < val:
                waits.append((sem, val))
                kn[sem] = val
        if dma is not None:
            sem = "dma_" + dma
            self.dma_sems.add(sem)
            inc = 16
            src = None
        else:
            sem = eng
            inc = 1
            src = eng
        self.count[sem] = self.count.get(sem, 0) + inc
        ev = (sem, self.count[sem], src)
        self.ops[eng].append((waits, fn, sem, inc))
        for k in writes:
            self.lastw[k] = ev
            self.readers[k] = {}
        for k in reads:
            self.readers.setdefault(k, {})[sem] = ev
        return ev


def build_program(phases=("gla", "x", "dsa", "epi"), taps=(), epi_branches=("gla", "dsa", "mem"),
                  dsa_iters=18, dsa_blocks=16, dsa_tap_blocks=()):
    nc = bass.Bass("TRN2", target_bir_lowering=False)
    s = Sch()
    es = ExitStack()
    st = {"stat_i": 0, "x_i": 0, "w_i": 0, "pb_i": 0, "pw_i": 0, "hc_i": 0}

    def dram_in(name, shape, dt=F32):
        return nc.dram_tensor(name, list(shape), dt, kind="ExternalInput").ap()

    def dram_out(name, shape, dt=F32):
        return nc.dram_tensor(name, list(shape), dt, kind="ExternalOutput").ap()

    def dram_tmp(name, shape, dt=F32):
        return nc.dram_tensor(name, list(shape), dt, kind="Internal").ap()

    def sb(name, shape, dt=F32):
        return es.enter_context(nc.sbuf_tensor(name, list(shape), dt))

    def psum(name, shape, dt=F32):
        return es.enter_context(nc.psum_tensor(name, list(shape), dt))

    x_own = dram_in("x_own", [NOWN, D])
    x_ctx = dram_in("x_ctx", [NCTX, D])
    w_in = dram_in("w_in", [D, W_IN_COLS])
    w_gla_out = dram_in("w_gla_out", [D, D])
    w_o = dram_in("w_o", [D, D])
    g_pre_d = dram_in("g_pre", [128, 8])
    g_post_d = dram_in("g_post", [128, D])
    g_gla_d = dram_in("g_gla", [128, 256])
    g_gla8_d = dram_in("g_gla8", [128, 8])
    waup_d = dram_in("w_a_up", [16, 512])
    ba_d = dram_in("b_a", [1, 512])
    ident_d = dram_in("ident", [128, 128], BF16)
    tribd_d = dram_in("tribd", [128, 128], BF16)
    trirev_d = dram_in("trirev", [128, 128], BF16)
    out = dram_out("out", [NOWN, D])
    mem_d = dram_in("mem", [256, D])
    w_mem_kv = dram_in("w_mem_kv", [D, 2048])
    w_dsa_out = dram_in("w_dsa_out", [D, D])
    w_x_out = dram_in("w_x_out", [D, D])
    g_mem_d = dram_in("g_mem", [128, 8])
    o_mem_d = dram_tmp("o_mem_scr", [NOWN, D], BF16)
    o_dsa_d = dram_tmp("o_dsa_scr", [NOWN, D], BF16)
    biasT_d = dram_in("biasT", [128, 2 * 8 * 128])
    cfarT_d = dram_in("cfarT", [128, 8 * 128])
    cmask_d = dram_in("cmask", [128, 128])
    ctxb_d = dram_in("ctxb", [128, 1])
    pow2_d = dram_in("pow2", [128, 32])
    bigI4_d = dram_in("bigI4", [128, 512], BF16)
    o_gla_d = dram_tmp("o_gla_scr", [NOWN, D], BF16)
    tapd = {}

    hT = sb("hT", [128, 8, NOWN], BF16)
    ident = sb("identsb", [128, 128], BF16)
    gpre = sb("gpre", [128, 8])
    xin = [sb("xin%d" % i, [128, D]) for i in range(2)]
    hb = [sb("hb%d" % i, [128, D], BF16) for i in range(2)]
    junk = sb("junk", [128, D], BF16)
    stat = sb("stat", [128, 64])
    wst = [sb("wst%d" % i, [128, 1024]) for i in range(4)]
    pw = [psum("pw%d" % i, [128, 1024]) for i in range(3)]
    pn = psum("pn0", [128, 512])
    ptr = psum("ptr", [128, 1024], BF16)

    def dma(ch, out_ap, in_ap, reads, writes, eng="sp"):
        s.op(eng, lambda e: e.dma_start(out=out_ap, in_=in_ap), reads=reads, writes=writes, dma=ch)

    def stat_cols(n=1):
        if st["stat_i"] % 64 + n > 64:
            st["stat_i"] += 64 - st["stat_i"] % 64
        i = st["stat_i"] % 64
        st["stat_i"] += n
        return i

    def pbank():
        i = 4 + st["pb_i"] % 3
        st["pb_i"] += 1
        if i == 6:
            return pn[:, :], [("pb", 6)]
        return pw[i // 2][:, (i % 2) * 512:(i % 2 + 1) * 512], [("pb", i)]

    def pwide():
        i = st["pw_i"] % 2
        st["pw_i"] += 1
        return pw[i], [("pb", 2 * i), ("pb", 2 * i + 1)]

    def run_merged_g(A, B):
        na, nb = len(A), len(B)
        ia = ib = 0
        while ia < na or ib < nb:
            if ib >= nb or (ia < na and ia * nb <= ib * na):
                A[ia]()
                ia += 1
            else:
                B[ib]()
                ib += 1

    def load_const(ch, dst, src, key):
        st["k_i"] = st.get("k_i", 0) + 1
        dma("k%d" % st["k_i"], dst, src, [], [key])

    load_const("c0", ident[:], ident_d, "ident")
    load_const("c1", gpre[:], g_pre_d, "gpre")
    ggl8 = sb("ggl8", [128, 8])
    load_const("c0", ggl8[:], g_gla8_d, "ggl8")

    def rstd_from_ss(c, n, dim):
        a = stat[:, c:c + n]
        keys = [("stat", c + j) for j in range(n)]
        s.op("act", lambda e: e.activation(out=a, in_=a, func=AF.Ln, scale=1.0 / dim, bias=EPS_T[:, 0:1]),
             reads=keys + ["eps"], writes=keys)
        s.op("act", lambda e: e.activation(out=a, in_=a, func=AF.Exp, scale=-0.5), reads=keys, writes=keys)

    EPS_T = sb("eps_t", [128, 2])
    s.op("pool", lambda e: e.memset(EPS_T[:, 0:1], EPS), writes=["eps"])
    s.op("pool", lambda e: e.memset(EPS_T[:, 1:2], 1.0), writes=["one"])

    def norm_block(x_dram, blk, dst, dst_slice, dst_key, split=False):
        i = st["x_i"] % 2
        st["x_i"] += 1
        xt, hbt = xin[i], hb[i]
        dma("x%d" % i, xt[:], x_dram[blk * 128:(blk + 1) * 128, :], [], [("xin", i)])
        c = stat_cols()
        ss = stat[:, c:c + 1]
        s.op("act", lambda e: e.activation(out=junk[:], in_=xt[:], func=AF.Square, accum_out=ss),
             reads=[("xin", i)], writes=["junk", ("stat", c)])
        rstd_from_ss(c, 1, D)
        s.op("dve", lambda e: e.tensor_scalar(out=hbt[:], in0=xt[:], scalar1=ss, scalar2=None, op0=ALU.mult),
             reads=[("xin", i), ("stat", c)], writes=[("hb", i)])

        def part_b():
            def tr(e):
                ins = None
                for kc in range(8):
                    ins = e.transpose(out=ptr[:, kc * 128:(kc + 1) * 128], in_=hbt[:, kc * 128:(kc + 1) * 128],
                                      identity=ident[:])
                return ins
            s.op("pe", tr, reads=[("hb", i), "ident"], writes=["ptr"])
            s.op("act", lambda e: e.activation(out=dst[:, :, dst_slice],
                                               in_=ptr[:, :].rearrange("p (k t) -> p k t", k=8), func=AF.Copy),
                 reads=["ptr"], writes=[dst_key])
        if split:
            return part_b
        part_b()

    def load_w_chunks(dst, dcol, wd, col_lo, ncols, key, scale=None, nk=8):
        T = []

        def one(kc, c0, n):
            i = st["w_i"] % 4
            st["w_i"] += 1
            dma("w%d" % i, wst[i][:, 0:n], wd[kc * 128:(kc + 1) * 128, col_lo + c0:col_lo + c0 + n],
                [], [("wst", i)])
            o_ap = dst[:, kc, dcol + c0:dcol + c0 + n]
            i_ap = wst[i][:, 0:n]
            use_act = (st["w_i"] % 2 == 0)
            if scale is not None:
                sc_ = scale[:, kc:kc + 1]
                if use_act:
                    s.op("act", lambda e: e.activation(out=o_ap, in_=i_ap, func=AF.Copy, scale=sc_),
                         reads=[("wst", i), "gpre", "gmem", "ggl8"], writes=[key])
                else:
                    s.op("dve", lambda e: e.tensor_scalar(out=o_ap, in0=i_ap, scalar1=sc_, scalar2=None, op0=ALU.mult),
                         reads=[("wst", i), "gpre", "gmem", "ggl8"], writes=[key])
            else:
                if use_act:
                    s.op("act", lambda e: e.activation(out=o_ap, in_=i_ap, func=AF.Copy), reads=[("wst", i)], writes=[key])
                else:
                    s.op("dve", lambda e: e.tensor_copy(out=o_ap, in_=i_ap), reads=[("wst", i)], writes=[key])
        for kc in range(nk):
            c0 = 0
            while c0 < ncols:
                n = min(1024, ncols - c0)
                T.append(lambda kc=kc, c0=c0, n=n: one(kc, c0, n))
                c0 += n
        return T

    def load_w(dst, dcol, wd, col_lo, ncols, key, scale=None, nk=8):
        for t_ in load_w_chunks(dst, dcol, wd, col_lo, ncols, key, scale, nk):
            t_()

    def mm_group(out_ap, pairs, reads, writes):
        def f(e):
            ins = None
            n = len(pairs)
            for j, (l, r) in enumerate(pairs):
                ins = e.matmul(out_ap, lhsT=l, rhs=r, start=(j == 0), stop=(j == n - 1))
            return ins
        s.op("pe", f, reads=reads, writes=writes)

    def tap(name, src_ap, shape, key, dt=F32):
        if name in taps:
            d = dram_out("tap_" + name, shape, dt)
            tapd[name] = d
            st["k_i"] = st.get("k_i", 0) + 1
            dma("k%d" % st["k_i"], d, src_ap, key if isinstance(key, list) else [key], [("tapd", name)])

    if "gla" not in phases:
        for blk in range(16):
            norm_block(x_own, blk, hT, slice(blk * 128, (blk + 1) * 128), ("hT", blk))
        tap("hT", hT[:, :, :], [128, 8, NOWN], ("hT", 15), BF16)

    xpre = None
    if "gla" in phases and "x" in phases:
        xw = ExitStack()
        Wkv = xw.enter_context(nc.sbuf_tensor("Wkv", [128, 8, 2048], BF16))
        Wxq = xw.enter_context(nc.sbuf_tensor("Wxq", [128, 8, 1024], BF16))
        gmem = xw.enter_context(nc.sbuf_tensor("gmem", [128, 8], F32))
        load_const("c0", gmem[:], g_mem_d, "gmem")
        xpre = load_w_chunks(Wkv, 0, w_mem_kv, 0, 2048, "Wkv", scale=gmem) + \
            load_w_chunks(Wxq, 0, w_in, COL["xq"], 1024, "Wxq", scale=gpre)
    if "gla" in phases:
        gl = ExitStack()

        def gsb(name, shape, dt=F32):
            return gl.enter_context(nc.sbuf_tensor(name, list(shape), dt))
        Wg = gsb("Wg", [128, 8, 2064], BF16)
        waup_f = gsb("waup_f", [16, 512])
        ba_f = gsb("ba_f", [1, 512])
        waup = gsb("waup", [16, 512], BF16)
        ba = gsb("ba", [1, 512], BF16)
        ones1 = gsb("ones1", [1, 128], BF16)
        tribd = gsb("tribd_sb", [128, 4, 128], BF16)
        trirev = gsb("trirev_sb", [128, 128], BF16)
        ggla = gsb("ggla", [128, 256])
        hTc = [gsb("hTc%d" % i, [128, 8, 128], BF16) for i in range(2)]
        gaT2 = [gsb("gaT%d" % i, [16, 128], BF16) for i in range(2)]
        e12 = [gsb("e1_%d" % i, [128, 512]) for i in range(2)]
        la2 = [gsb("la%d" % i, [128, 512], BF16) for i in range(2)]
        Eq2 = [gsb("Eq%d" % i, [128, 512]) for i in range(2)]
        Ek2 = [gsb("Ek%d" % i, [128, 512]) for i in range(2)]
        Er2 = [gsb("Er%d" % i, [128, 512]) for i in range(2)]
        dec2 = [gsb("dec%d" % i, [128, 4, 2]) for i in range(2)]
        qdA2 = [gsb("qdA%d" % i, [128, 4, 128], BF16) for i in range(2)]
        qdB2 = [gsb("qdB%d" % i, [128, 4, 128], BF16) for i in range(2)]
        kd2 = [gsb("kd%d" % i, [128, 4, 128], BF16) for i in range(2)]
        kt2 = [gsb("kt%d" % i, [128, 512], BF16) for i in range(2)]
        vv2 = [gsb("vv%d" % i, [128, 1024], BF16) for i in range(2)]
        attT2 = [gsb("attT%d" % i, [128, 4, 128], BF16) for i in range(2)]
        Sf = [gsb("Sf%d" % i, [128, 4, 256]) for i in range(2)]
        Sb = [gsb("Sb%d" % i, [128, 4, 256], BF16) for i in range(2)]
        og = [gsb("og%d" % i, [128, 1024], BF16) for i in range(2)]

        load_w(Wg, 0, w_in, COL["gq"], 2064, "Wg", scale=gpre)
        load_const("c0", waup_f[:], waup_d, "waup_f")
        load_const("c1", ba_f[:], ba_d, "ba_f")
        s.op("act", lambda e: e.activation(out=waup[:], in_=waup_f[:], func=AF.Copy), reads=["waup_f"], writes=["waup"])
        s.op("act", lambda e: e.activation(out=ba[:], in_=ba_f[:], func=AF.Copy), reads=["ba_f"], writes=["ba"])
        s.op("pool", lambda e: e.memset(ones1[:], 1.0), writes=["ones1"])
        for h in range(4):
            load_const("c0", tribd[:, h, :], tribd_d, ("tribd", h))
        load_const("c1", trirev[:], trirev_d, "trirev")
        load_const("c0", ggla[:], g_gla_d, "ggla")
        s.op("pool", lambda e: e.memset(Sf[0][:], 0.0), writes=[("Sf", 0)])
        s.op("pool", lambda e: e.memset(Sb[0][:], 0.0), writes=[("Sb", 0)])
        for i_ in range(2):
            s.op("pool", lambda e, i_=i_: e.memset(qdA2[i_][:], 0.0), writes=[("qdA", i_)])
            s.op("pool", lambda e, i_=i_: e.memset(qdB2[i_][:], 0.0), writes=[("qdB", i_)])
        gst = {"n": 0}
        tribd_keys = [("tribd", h) for h in range(4)]

        def gla_block(src, sl, src_key, own, oblk):
            hs = lambda kc: src[:, kc, sl]
            par = gst["n"] % 2
            gst["n"] += 1
            gaT, e1, la, Eq, Ek, Er, dec = gaT2[par], e12[par], la2[par], Eq2[par], Ek2[par], Er2[par], dec2[par]
            qdA, qdB, kd, kt, vv, attT = qdA2[par], qdB2[par], kd2[par], kt2[par], vv2[par], attT2[par]
            K_ = lambda n_: (n_, par)
            def f1():
                P, pk = pbank()
                mm_group(P[0:16, 0:128], [(Wg[:, kc, 2048:2064], hs(kc)) for kc in range(8)],
                         [src_key, "Wg"], pk)
                s.op("act", lambda e: e.activation(out=gaT[:], in_=P[0:16, 0:128], func=AF.Copy),
                     reads=pk, writes=[K_("gaT")])
            def f2():
                P2, pk2 = pbank()
                mm_group(P2, [(gaT[0:16, :], waup[0:16, :]), (ones1[0:1, :], ba[0:1, :])],
                         [K_("gaT"), "waup", "ba", "ones1"], pk2)
                s.op("act", lambda e: e.activation(out=e1[:], in_=P2, func=AF.Exp, scale=-1.0),
                     reads=pk2, writes=[K_("e1")])
                s.op("act", lambda e: e.activation(out=la[:], in_=e1[:], func=AF.Ln, bias=EPS_T[:, 1:2]),
                     reads=[K_("e1"), "one"], writes=[K_("la")])
            def f3():
                P3, pk3 = pbank()
                mm_group(P3, [(trirev[:, :], la[:, :])], ["trirev", K_("la")], pk3)
                s.op("act", lambda e: e.activation(out=Er[:], in_=P3, func=AF.Exp, scale=-1.0 / 16),
                     reads=pk3, writes=[K_("Er")])
            def f4():
                P4, pk4 = pbank()

                def fc(e):
                    ins = None
                    for h in range(4):
                        ins = e.matmul(P4[:, h * 128:(h + 1) * 128], lhsT=la[:, h * 128:(h + 1) * 128],
                                       rhs=tribd[:, 0, :], start=True, stop=True)
                    return ins
                s.op("pe", fc, reads=[K_("la")] + tribd_keys, writes=pk4)
                P4v = P4.rearrange("p (h c t) -> p h c t", h=4, c=2)
                s.op("act", lambda e: e.activation(out=dec[:], in_=P4v[:, :, :, 63], func=AF.Exp, scale=-1.0 / 16),
                     reads=pk4, writes=[K_("dec")])
                if own:
                    s.op("act", lambda e: e.activation(out=Eq[:], in_=P4, func=AF.Exp, scale=-1.0 / 16),
                         reads=pk4, writes=[K_("Eq")])
                    s.op("act", lambda e: e.activation(out=Ek[:], in_=P4, func=AF.Exp, scale=1.0 / 16),
                         reads=pk4, writes=[K_("Ek")])
            def b1():
                P5, pk5 = pbank()
                mm_group(P5, [(hs(kc), Wg[:, kc, 512:1024]) for kc in range(8)], [src_key, "Wg"], pk5)
                s.op("dve", lambda e: e.tensor_tensor(out=kt[:], in0=P5, in1=Er[:], op=ALU.mult),
                     reads=pk5 + [K_("Er")], writes=[K_("kt")])
            def b2():
                PV, pkv = pwide()
                for half in range(2):
                    mm_group(PV[:, half * 512:(half + 1) * 512],
                             [(hs(kc), Wg[:, kc, 1024 + half * 512:1024 + (half + 1) * 512]) for kc in range(8)],
                             [src_key, "Wg"], [pkv[half]])
                s.op("act", lambda e: e.activation(out=vv[:], in_=PV[:, :], func=AF.Copy), reads=pkv, writes=[K_("vv")])
            def b3():
                P6, pk6 = pbank()

                def fq(e):
                    ins = None
                    for h in range(4):
                        for kc in range(8):
                            ins = e.matmul(P6[:, h * 128:(h + 1) * 128], lhsT=Wg[:, kc, h * 128:(h + 1) * 128],
                                           rhs=hs(kc), start=(kc == 0), stop=(kc == 7))
                    return ins
                s.op("pe", fq, reads=[src_key, "Wg"], writes=pk6)
                P6v = P6.rearrange("p (h t) -> p h t", h=4)
                Eqv = Eq[:].rearrange("p (h t) -> p h t", h=4)
                s.op("dve", lambda e: e.scalar_tensor_tensor(
                    out=qdA[:, :, 0:64], in0=P6v[:, :, 0:64], scalar=128 ** -0.5, in1=Eqv[:, :, 0:64],
                    op0=ALU.mult, op1=ALU.mult), reads=pk6 + [K_("Eq")], writes=[K_("qdA")])
                s.op("dve", lambda e: e.scalar_tensor_tensor(
                    out=qdB[:, :, 64:128], in0=P6v[:, :, 64:128], scalar=128 ** -0.5, in1=Eqv[:, :, 64:128],
                    op0=ALU.mult, op1=ALU.mult), reads=pk6 + [K_("Eq")], writes=[K_("qdB")])
            def b4():
                P7, pk7 = pbank()

                def fk(e):
                    ins = None
                    for h in range(4):
                        for kc in range(8):
                            ins = e.matmul(P7[:, h * 128:(h + 1) * 128],
                                           lhsT=Wg[:, kc, 512 + h * 128:512 + (h + 1) * 128],
                                           rhs=hs(kc), start=(kc == 0), stop=(kc == 7))
                    return ins
                s.op("pe", fk, reads=[src_key, "Wg"], writes=pk7)
                s.op("dve", lambda e: e.tensor_tensor(out=kd[:].rearrange("p h t -> p (h t)"), in0=P7, in1=Ek[:],
                                                      op=ALU.mult), reads=pk7 + [K_("Ek")], writes=[K_("kd")])
            def b5():
                P8, pk8 = pbank()

                def fa(e):
                    ins = None
                    for h in range(4):
                        e.matmul(P8[:, h * 128:(h + 1) * 128], lhsT=kd[:, h, :], rhs=qdA[:, h, :],
                                 start=True, stop=False)
                        ins = e.matmul(P8[:, h * 128:(h + 1) * 128], lhsT=kd[:, h, :], rhs=qdB[:, h, :],
                                       start=False, stop=True)
                    return ins
                s.op("pe", fa, reads=[K_("kd"), K_("qdA"), K_("qdB")], writes=pk8)
                s.op("dve", lambda e: e.tensor_tensor(out=attT[:].rearrange("p h t -> p (h t)"), in0=P8,
                                                      in1=tribd[:].rearrange("p h t -> p (h t)"), op=ALU.mult),
                     reads=pk8 + tribd_keys, writes=[K_("attT")])

            def state_update(c, s_in, s_out):
                PU, pku = pwide()

                def fu(e):
                    ins = None
                    for h in range(4):
                        ins = e.matmul(PU[:, h * 256:(h + 1) * 256], lhsT=kt[c * 64:(c + 1) * 64, h * 128:(h + 1) * 128],
                                       rhs=vv[c * 64:(c + 1) * 64, h * 256:(h + 1) * 256], start=True, stop=True)
                    return ins
                s.op("pe", fu, reads=[K_("kt"), K_("vv")], writes=pku)
                for h in range(4):
                    s.op("dve", lambda e, h=h: e.scalar_tensor_tensor(
                        out=Sf[s_out][:, h, :], in0=Sf[s_in][:, h, :], scalar=dec[:, h, c:c + 1],
                        in1=PU[:, h * 256:(h + 1) * 256], op0=ALU.mult, op1=ALU.add),
                        reads=pku + [("Sf", s_in), K_("dec")], writes=[("Sf", s_out)])
                s.op("dve", lambda e: e.tensor_copy(out=Sb[s_out][:], in_=Sf[s_out][:]),
                     reads=[("Sf", s_out)], writes=[("Sb", s_out)])

            def b7():
                PO, pko = pwide()

                def fo(e):
                    ins = None
                    for h in range(4):
                        o_ap = PO[:, h * 256:(h + 1) * 256]
                        e.matmul(o_ap, lhsT=attT[:, h, :], rhs=vv[:, h * 256:(h + 1) * 256], start=True, stop=False)
                        e.matmul(o_ap, lhsT=qdA[:, h, :], rhs=Sb[0][:, h, :], start=False, stop=False)
                        ins = e.matmul(o_ap, lhsT=qdB[:, h, :], rhs=Sb[1][:, h, :], start=False, stop=True)
                    return ins
                s.op("pe", fo, reads=[K_("attT"), K_("vv"), K_("qdA"), K_("qdB"), ("Sb", 0), ("Sb", 1)], writes=pko)
                c = stat_cols(4)
                for h in range(4):
                    s.op("act", lambda e, h=h: e.activation(out=junk[:, h * 256:(h + 1) * 256], in_=PO[:, h * 256:(h + 1) * 256],
                                                            func=AF.Square, accum_out=stat[:, c + h:c + h + 1]),
                         reads=pko, writes=["junk", ("stat", c + h)])
                rstd_from_ss(c, 4, 256)
                ogt = og[oblk % 2]
                for h in range(4):
                    s.op("act", lambda e, h=h: e.activation(
                        out=ogt[:, h * 256:(h + 1) * 256], in_=PO[:, h * 256:(h + 1) * 256], func=AF.Copy,
                        scale=stat[:, c + h:c + h + 1]),
                        reads=pko + [("stat", c + h)], writes=[("og", oblk % 2)])
                dma("og%d" % (oblk % 2), o_gla_d[oblk * 128:(oblk + 1) * 128, :], ogt[:],
                    [("og", oblk % 2)], [("o_gla", oblk)])
            front = [f1, f2, f3, f4]
            back = [b1, b2]
            if own:
                back += [b3, b4, b5]
            back.append(lambda: state_update(0, 0, 1))
            if own:
                back.append(b7)
            back.append(lambda: state_update(1, 1, 0))
            return front, back

        prev_back = []
        for blk in range(32):
            if blk < 16:
                i = st["hc_i"] % 2
                st["hc_i"] += 1
                norm_block(x_ctx, blk, hTc[i], slice(0, 128), ("hTc", i))
                norm_block(x_own, blk, hT, slice(blk * 128, (blk + 1) * 128), ("hT", blk))
                front, back = gla_block(hTc[i], slice(0, 128), ("hTc", i), False, None)
            else:
                ob = blk - 16
                front, back = gla_block(hT, slice(ob * 128, (ob + 1) * 128), ("hT", ob), True, ob)
            run_merged_g(prev_back, front)
            prev_back = back
            if xpre and blk >= 6:
                xpre.pop(0)()
        run_merged_g(prev_back, [])
        if "mem" in _os_environ_get("DBG", ""):
            print("GLA sbuf remaining", nc.sbuf_bytes_remaining)
        gl.close()
        s.barrier()
        if "o_gla" in taps:
            d = dram_out("tap_o_gla", [NOWN, D], BF16)
            tapd["o_gla"] = d
            dma("tapg", d, o_gla_d, [("o_gla", b_) for b_ in range(16)], [("tapd", "o_gla")])


    if "x" in phases:
        xl = ExitStack()

        def xsb(name, shape, dt=F32):
            return xl.enter_context(nc.sbuf_tensor(name, list(shape), dt))
        if xpre is None:
            Wkv = xsb("Wkv", [128, 8, 2048], BF16)
            Wxq = xsb("Wxq", [128, 8, 1024], BF16)
            gmem = xsb("gmem", [128, 8])
        memT = xsb("memT", [128, 8, 256], BF16)
        mkT = xsb("mkT", [128, 8, 256], BF16)
        mv1 = xsb("mv1", [128, 2, 4, 256], BF16)
        onesc = xsb("onesc", [128, 2], BF16)
        xqT = xsb("xqT", [128, 8, 512], BF16)
        PT = [[xsb("PT%d_%d" % (h, m), [128, 512], BF16) for m in range(2)] for h in range(4)]
        rs = xsb("rsx", [128, 4])
        om = [xsb("om%d" % i, [128, 1024], BF16) for i in range(2)]
        s.op("pool", lambda e: e.memset(onesc[:], 1.0), writes=["onesc"])
        if xpre is None:
            load_const("c0", gmem[:], g_mem_d, "gmem")
            load_w(Wkv, 0, w_mem_kv, 0, 2048, "Wkv", scale=gmem)
            load_w(Wxq, 0, w_in, COL["xq"], 1024, "Wxq", scale=gpre)
        else:
            while xpre:
                xpre.pop(0)()
        for mb in range(2):
            norm_block(mem_d, mb, memT, slice(mb * 128, (mb + 1) * 128), ("memT", mb))
        memk = [("memT", 0), ("memT", 1)]
        for hd in range(8):
            P, pk = pbank()
            mm_group(P[:, 0:256], [(Wkv[:, kc, hd * 128:(hd + 1) * 128], memT[:, kc, :]) for kc in range(8)],
                     memk + ["Wkv"], pk)
            s.op("act", lambda e, P=P, hd=hd: e.activation(out=mkT[:, hd, :], in_=P[:, 0:256], func=AF.Copy),
                 reads=pk, writes=["mkT"])
        for mb in range(2):
            for half in range(2):
                P, pk = pbank()
                mm_group(P, [(memT[:, kc, mb * 128:(mb + 1) * 128],
                              Wkv[:, kc, 1024 + half * 512:1024 + (half + 1) * 512]) for kc in range(8)],
                         memk + ["Wkv"], pk)
                s.op("act", lambda e, P=P, mb=mb, half=half: e.activation(
                    out=mv1[:, mb, 2 * half:2 * half + 2, :].rearrange("p h d -> p (h d)"), in_=P, func=AF.Copy),
                    reads=pk, writes=["mv1"])
        for sbk in range(4):
            tsl = slice(sbk * 512, (sbk + 1) * 512)
            hkeys = [("hT", sbk * 4 + j) for j in range(4)]
            for hd in range(8):
                P, pk = pbank()
                mm_group(P, [(Wxq[:, kc, hd * 128:(hd + 1) * 128], hT[:, kc, tsl]) for kc in range(8)],
                         hkeys + ["Wxq"], pk)
                s.op("act", lambda e, P=P, hd=hd: e.activation(out=xqT[:, hd, :], in_=P, func=AF.Copy, scale=1.0 / 16),
                     reads=pk, writes=[("xqT", hd)])
            for h in range(4):
                for mb in range(2):
                    P, pk = pbank()
                    mm_group(P, [(mkT[:, 2 * h + dc, mb * 128:(mb + 1) * 128], xqT[:, 2 * h + dc, :]) for dc in range(2)],
                             ["mkT", ("xqT", 2 * h), ("xqT", 2 * h + 1)], pk)
                    s.op("act", lambda e, P=P, h=h, mb=mb: e.activation(out=PT[h][mb][:], in_=P, func=AF.Exp),
                         reads=pk, writes=[("PT", h, mb)])
            for j in range(4):
                blk = sbk * 4 + j
                PO, pko = pwide()
                PS, pks = pbank()

                def fx(e, PO=PO, PS=PS, j=j):
                    ins = None
                    for h in range(4):
                        for mb in range(2):
                            e.matmul(PO[:, h * 256:(h + 1) * 256], lhsT=PT[h][mb][:, j * 128:(j + 1) * 128],
                                     rhs=mv1[:, mb, h, :], start=(mb == 0), stop=(mb == 1))
                    for h in range(4):
                        for mb in range(2):
                            ins = e.matmul(PS[:, 2 * h:2 * h + 2], lhsT=PT[h][mb][:, j * 128:(j + 1) * 128],
                                           rhs=onesc[:, 0:2], start=(mb == 0), stop=(mb == 1))
                    return ins
                s.op("pe", fx, reads=[("PT", h, mb) for h in range(4) for mb in range(2)] + ["mv1", "onesc"],
                     writes=pko + pks)
                s.op("dve", lambda e, PS=PS: e.reciprocal(out=rs[:], in_=PS[:, 0:8].rearrange("p (h two) -> p h two", two=2)[:, :, 0]),
                     reads=pks, writes=["rsx"])
                omt = om[blk % 2]
                for h in range(4):
                    s.op("act", lambda e, h=h, PO=PO, omt=omt: e.activation(
                        out=omt[:, h * 256:(h + 1) * 256], in_=PO[:, h * 256:(h + 1) * 256], func=AF.Copy,
                        scale=rs[:, h:h + 1]), reads=pko + ["rsx"], writes=[("om", blk % 2)])
                dma("om%d" % (blk % 2), o_mem_d[blk * 128:(blk + 1) * 128, :], omt[:],
                    [("om", blk % 2)], [("o_mem", blk)])
        xl.close()
        if "gla" in phases:
            xw.close()
        s.barrier()
        if "o_mem" in taps:
            d = dram_out("tap_o_mem", [NOWN, D], BF16)
            tapd["o_mem"] = d
            dma("tapm", d, o_mem_d, [("o_mem", b_) for b_ in range(16)], [("tapd", "o_mem")])


    if "dsa" in phases:
        NIT = dsa_iters
        dl = ExitStack()

        def dsb(name, shape, dt=F32):
            return dl.enter_context(nc.sbuf_tensor(name, list(shape), dt))
        KT = dsb("KT", [128, 2, NTOK], BF16)
        V1 = dsb("V1", [128, 32, 2, 129], BF16)
        ikT2 = dsb("ikT2", [128, NTOK], BF16)
        biasTb = dsb("biasTb", [128, 2, 8, 128], BF16)
        cmask = dsb("cmask_sb", [128, 128])
        ctxb = dsb("ctxb_sb", [128, 1])
        pow2 = dsb("pow2_sb", [128, 32])
        onesd = dsb("onesd", [128, 2], BF16)
        bigI4 = dsb("bigI4_sb", [128, 512], BF16)
        hTc2 = [dsb("hTd%d" % i, [128, 8, 128], BF16) for i in range(2)]
        scb = [dsb("sc%d" % i, [128, NTOK]) for i in range(2)]
        bst = scb[0][:, 0:2048].rearrange("p (a h t) -> p a h t", a=2, h=8)
        cst_ = scb[0][:, 2048:3072].rearrange("p (h t) -> p h t", h=8)
        load_const("c0", scb[0][:, 0:2048], biasT_d, "bst")
        load_const("c1", scb[0][:, 2048:3072], cfarT_d, "cst")
        load_const("c0", cmask[:], cmask_d, "cmask")
        load_const("c1", ctxb[:], ctxb_d, "ctxb")
        load_const("c0", pow2[:], pow2_d, "pow2")
        load_const("c1", bigI4[:], bigI4_d, "bigI4")
        s.op("pool", lambda e: e.memset(onesd[:], 1.0), writes=["onesd"])
        s.op("pool", lambda e: e.memset(V1[:, :, :, 128:129], 1.0), writes=["V1ones"])
        for a in range(2):
            s.op("dve", lambda e, a=a: e.tensor_tensor(out=biasTb[:, a], in0=bst[:, a], in1=cst_, op=ALU.subtract),
                 reads=["bst", "cst"], writes=["biasTb", ("sc", 0)])
        dst = {"i": 0}

        def dbank():
            i = 3 + dst["i"] % 4
            dst["i"] += 1
            if i == 6:
                return pn[:, :], [("pb", 6)]
            return pw[i // 2][:, (i % 2) * 512:(i % 2 + 1) * 512], [("pb", i)]

        kl = ExitStack()
        Wkv2 = kl.enter_context(nc.sbuf_tensor("Wkv2", [128, 8, 640], BF16))
        load_w(Wkv2, 0, w_in, COL["dk"], 512, "Wkv2", scale=gpre)
        load_w(Wkv2, 512, w_in, COL["ik"], 64, "Wkv2", scale=gpre)
        load_w(Wkv2, 576, w_in, COL["ik"], 64, "Wkv2", scale=gpre)
        norm_block(x_ctx, 0, hTc2[0], slice(0, 128), ("hTd", 0))
        for kb in range(32):
            nb_late = None
            if kb < 16:
                i = kb % 2
                if kb + 1 < 16:
                    nb_late = norm_block(x_ctx, kb + 1, hTc2[(kb + 1) % 2], slice(0, 128), ("hTd", (kb + 1) % 2), split=True)
                src_, sl_, skey = hTc2[i], slice(0, 128), ("hTd", i)
            else:
                src_, sl_, skey = hT, slice((kb - 16) * 128, (kb - 15) * 128), ("hT", kb - 16)
            P, pk = dbank()

            def fkk(e, P=P, src_=src_, sl_=sl_):
                ins = None
                for g in range(2):
                    for kc in range(8):
                        ins = e.matmul(P[:, g * 128:(g + 1) * 128], lhsT=Wkv2[:, kc, g * 128:(g + 1) * 128],
                                       rhs=src_[:, kc, sl_], start=(kc == 0), stop=(kc == 7))
                for kc in range(8):
                    ins = e.matmul(P[:, 256:384], lhsT=Wkv2[:, kc, 512:640], rhs=src_[:, kc, sl_],
                                   start=(kc == 0), stop=(kc == 7))
                return ins
            s.op("pe", fkk, reads=[skey, "Wkv2"], writes=pk)
            s.op("act", lambda e, P=P, kb=kb: e.activation(
                out=KT[:, :, kb * 128:(kb + 1) * 128], in_=P[:, 0:256].rearrange("p (g t) -> p g t", g=2), func=AF.Copy),
                reads=pk, writes=[("KT", kb)])
            s.op("dve", lambda e, P=P, kb=kb: e.tensor_copy(out=ikT2[:, kb * 128:(kb + 1) * 128], in_=P[:, 256:384]),
                 reads=pk, writes=[("ikT", kb)])
            P2, pk2 = dbank()
            mm_group(P2[:, 0:256], [(src_[:, kc, sl_], Wkv2[:, kc, 256:512]) for kc in range(8)], [skey, "Wkv2"], pk2)
            s.op("act", lambda e, P2=P2, kb=kb: e.activation(out=V1[:, kb, :, 0:128],
                                                            in_=P2[:, 0:256].rearrange("p (g d) -> p g d", g=2), func=AF.Copy),
                 reads=pk2 + ["V1ones"], writes=[("V1", kb)])
            if nb_late is not None:
                nb_late()
        kl.close()
        s.barrier()
        if "KT" in taps:
            tap("KT", KT[:, :, :], [128, 2, NTOK], [("KT", kb) for kb in range(32)], BF16)
            tap("V1", V1[:, :, :, :], [128, 32, 2, 129], [("V1", kb) for kb in range(32)], BF16)
            tap("ikT2", ikT2[:, :], [128, NTOK], [("ikT", kb) for kb in range(32)], BF16)

        Wq = dsb("Wq", [128, 8, 1544], BF16)
        load_w(Wq, 0, w_in, COL["dq"], 1024, "Wq", scale=gpre)
        load_w(Wq, 1024, w_in, COL["iq"], 512, "Wq", scale=gpre)
        load_w(Wq, 1536, w_in, COL["iw"], 8, "Wq", scale=gpre)
        QTb = [dsb("QT%d" % i, [128, 8, 128], BF16) for i in range(3)]
        iqT2 = dsb("iqT2", [128, 4, 128], BF16)
        iwb = dsb("iwb", [128, 8])
        Dg = dsb("Dg", [128, 8, 128], BF16)
        rl = [dsb("rl%d" % i, [128, 512], BF16) for i in range(3)]
        Mnb = [dsb("Mn%d" % i, [128, NTOK], BF16) for i in range(2)]
        PTd = [dsb("PTd%d" % i, [128, 512], BF16) for i in range(3)]
        bs = dsb("bis", [128, 40])
        rsd = dsb("rsd", [128, 8])
        od = [dsb("od%d" % i, [128, 1024], BF16) for i in range(2)]
        ISC = (8 ** -0.5) * (64 ** -0.5)
        if "mem" in _os_environ_get("DBG", ""):
            print("DSA sbuf remaining", nc.sbuf_bytes_remaining)
        PACC = ptr[:, :].bitcast(F32)
        sst = {"s": 0, "a": 0, "r": 0}

        def sbank():
            i = 5 + sst["s"] % 2
            sst["s"] += 1
            if i == 6:
                return pn[:, :], [("pb", 6)]
            return pw[2][:, 512:1024], [("pb", 5)]

        def abank():
            i = 3 + sst["a"] % 2
            sst["a"] += 1
            if i == 3:
                return pw[1][:, 512:1024], [("pb", 3)]
            return pw[2][:, 0:512], [("pb", 4)]

        def score_thunks(j):
            T = []
            hsl = slice(j * 128, (j + 1) * 128)
            hk = ("hT", j)
            nkb = 17 + j
            N = nkb * 128
            QTj = QTb[j % 3]
            scj = scb[j % 2]

            def t_q(g):
                P, pk = sbank()

                def fq(e):
                    ins = None
                    for hh in range(4):
                        h = 4 * g + hh
                        for kc in range(8):
                            ins = e.matmul(P[:, hh * 128:(hh + 1) * 128], lhsT=Wq[:, kc, h * 128:(h + 1) * 128],
                                           rhs=hT[:, kc, hsl], start=(kc == 0), stop=(kc == 7))
                    return ins
                s.op("pe", fq, reads=[hk, "Wq"], writes=pk)
                s.op("act", lambda e: e.activation(
                    out=QTj[:, 4 * g:4 * g + 4, :].rearrange("p h t -> p (h t)"), in_=P, func=AF.Copy, scale=128 ** -0.5),
                    reads=pk, writes=[("QT", j % 3, g)])
            T.append(lambda: t_q(0))
            T.append(lambda: t_q(1))

            def t_iq():
                P, pk = sbank()

                def fiq(e):
                    ins = None
                    for c in range(4):
                        for kc in range(8):
                            ins = e.matmul(P[:, c * 128:(c + 1) * 128], lhsT=Wq[:, kc, 1024 + c * 128:1024 + (c + 1) * 128],
                                           rhs=hT[:, kc, hsl], start=(kc == 0), stop=(kc == 7))
                    return ins
                s.op("pe", fiq, reads=[hk, "Wq"], writes=pk)
                s.op("act", lambda e: e.activation(out=iqT2[:].rearrange("p c t -> p (c t)"), in_=P, func=AF.Copy),
                     reads=pk, writes=["iqT2"])
                P2, pk2 = sbank()
                mm_group(P2[:, 0:8], [(hT[:, kc, hsl], Wq[:, kc, 1536:1544]) for kc in range(8)], [hk, "Wq"], pk2)
                s.op("act", lambda e: e.activation(out=iwb[:], in_=P2[:, 0:8], func=AF.Copy, scale=ISC),
                     reads=pk2, writes=["iwb"])
                for h in range(8):
                    s.op("act", lambda e, h=h: e.activation(out=Dg[:, h, :], in_=ident[:], func=AF.Copy,
                                                            scale=iwb[:, h:h + 1]),
                         reads=["ident", "iwb"], writes=[("Dg", h)])
            T.append(t_iq)
            nch = (N + 511) // 512
            sckeys = [("sc", j % 2, i_) for i_ in range(nch)]
            pend = {}

            def rec_s(ci, h):
                c0 = ci * 512
                n = min(512, N - c0)
                kkeys = [("ikT", kb) for kb in range(c0 // 128, (c0 + n) // 128)]
                P, pk = sbank()
                pr = (h % 2) * 64
                s.op("pe", lambda e: e.matmul(P[:, 0:n], lhsT=iqT2[pr:pr + 64, h // 2, :], rhs=ikT2[pr:pr + 64, c0:c0 + n],
                                              start=True, stop=True), reads=["iqT2"] + kkeys, writes=pk)
                pend[(ci, h)] = (P, pk)

            def rec_acc(ci, h):
                c0 = ci * 512
                n = min(512, N - c0)
                P, pk = pend.pop((ci, h))
                ri = sst["r"] % 3
                sst["r"] += 1
                r_ = rl[ri]
                s.op("act", lambda e: e.activation(out=r_[:, 0:n], in_=P[:, 0:n], func=AF.Relu),
                     reads=pk, writes=[("rl", ri)])
                s.op("pe", lambda e: e.matmul(PACC[:, 0:n], lhsT=Dg[:, h, :], rhs=r_[:, 0:n], start=(h == 0), stop=(h == 7),
                                              skip_group_check=True),
                     reads=[("rl", ri), ("Dg", h)], writes=["ptr"])
                if h == 7:
                    s.op("act", lambda e: e.activation(out=scj[:, c0:c0 + n], in_=PACC[:, 0:n], func=AF.Copy),
                         reads=["ptr"], writes=[("sc", j % 2, ci)])
            seq = [(ci, h) for ci in range(nch) for h in range(8)]

            def t_step(k_):
                if k_ == 0:
                    rec_s(*seq[0])
                if k_ + 1 < len(seq):
                    rec_s(*seq[k_ + 1])
                rec_acc(*seq[k_])
            for k_ in range(len(seq)):
                T.append(lambda k_=k_: t_step(k_))
            return T

        def bis_thunks(j):
            T = []
            nkb = 17 + j
            N = nkb * 128
            scj = scb[j % 2]
            Mn = Mnb[j % 2]
            nch = (N + 511) // 512
            sckeys = [("sc", j % 2, i_) for i_ in range(nch)]
            mkey = ("Mn", j % 2)

            def t_prep():
                s.op("dve", lambda e: e.tensor_reduce(out=bs[:, 0:1], in_=scj[:, 0:N], axis=AX.X, op=ALU.max,
                                                      apply_absolute_value=True), reads=sckeys, writes=["bs_R"])
                s.op("dve", lambda e: e.tensor_scalar(out=bs[:, 0:1], in0=bs[:, 0:1], scalar1=1.01, scalar2=1e-6,
                                                      op0=ALU.mult, op1=ALU.add), reads=["bs_R"], writes=["bs_R"])
                s.op("dve", lambda e: e.tensor_scalar(out=bs[:, 8:8 + NIT + 1], in0=pow2[:, 0:NIT + 1], scalar1=bs[:, 0:1],
                                                      scalar2=None, op0=ALU.mult), reads=["bs_R", "pow2"], writes=["bs_w"])
                s.op("dve", lambda e: e.tensor_scalar(out=scj[:, 0:NCTX], in0=scj[:, 0:NCTX], scalar1=ctxb[:, 0:1],
                                                      scalar2=None, op0=ALU.add),
                     reads=sckeys + ["ctxb", "bs_R"], writes=sckeys)
                s.op("dve", lambda e: e.tensor_tensor(out=scj[:, N - 128:N], in0=scj[:, N - 128:N], in1=cmask[:], op=ALU.add),
                     reads=sckeys + ["cmask", "bs_R"], writes=sckeys)
                s.op("dve", lambda e: e.memset(bs[:, 1:2], 0.0), writes=["bs_mid"])
            T.append(t_prep)

            def t_bis(it):
                s.op("dve", lambda e: e.tensor_scalar(out=Mn[:, 0:N], in0=scj[:, 0:N], scalar1=bs[:, 1:2], scalar2=None,
                                                      op0=ALU.is_ge, op1=ALU.add, accum_out=bs[:, 2:3]),
                     reads=sckeys + ["bs_mid"], writes=[mkey, "bs_cnt"])
                s.op("dve", lambda e: e.tensor_scalar(out=bs[:, 3:4], in0=bs[:, 2:3], scalar1=255.5,
                                                      scalar2=bs[:, 8 + it:9 + it], op0=ALU.is_ge, op1=ALU.mult),
                     reads=["bs_cnt", "bs_w"], writes=["bs_g"])
                s.op("dve", lambda e: e.scalar_tensor_tensor(out=bs[:, 1:2], in0=bs[:, 3:4],
                                                             scalar=bs[:, 9 + it:10 + it], in1=bs[:, 1:2],
                                                             op0=ALU.subtract, op1=ALU.add),
                     reads=["bs_g", "bs_w", "bs_mid"], writes=["bs_mid"])
            for it in range(NIT):
                T.append(lambda it=it: t_bis(it))

            def t_mask():
                s.op("dve", lambda e: e.tensor_tensor(out=bs[:, 4:5], in0=bs[:, 1:2], in1=bs[:, 8 + NIT:9 + NIT],
                                                      op=ALU.subtract), reads=["bs_mid", "bs_w"], writes=["bs_lo"])
                s.op("dve", lambda e: e.tensor_scalar(out=Mn[:, 0:N], in0=scj[:, 0:N], scalar1=bs[:, 4:5], scalar2=1.0,
                                                      op0=ALU.is_ge, op1=ALU.subtract), reads=sckeys + ["bs_lo"],
                     writes=[mkey])
                if j in dsa_tap_blocks:
                    tap("sc%d" % j, scj[:, 0:N], [128, N], sckeys)
                    tap("Mm%d" % j, Mn[:, 0:N], [128, N], [mkey], BF16)
            T.append(t_mask)
            return T

        PObanks = [pw[0][:, 0:512], pw[0][:, 512:1024], pw[1][:, 0:512]]
        pokeys = [("pb", 0), ("pb", 1), ("pb", 2)]

        def attn_thunks(j):
            T = []
            nkb = 17 + j
            QTj = QTb[j % 3]
            Mn = Mnb[j % 2]
            mkey = ("Mn", j % 2)
            items = [(g, kb) for g in range(2) for kb in range(nkb)]
            nit_ = len(items)
            qkb = {}

            def rec_qk(i_):
                g, kb = items[i_]
                delta = 16 + j - kb
                P, pk = abank()

                def fqk(e):
                    e.matmul(P, lhsT=KT[:, g, kb * 128:(kb + 1) * 128],
                             rhs=QTj[:, 4 * g:4 * g + 4, :].rearrange("p h t -> p (h t)"), start=True, stop=False)
                    if delta < 2:
                        e.matmul(P, lhsT=ident[:], rhs=biasTb[:, delta, 4 * g:4 * g + 4, :].rearrange("p h t -> p (h t)"),
                                 start=False, stop=False)
                    return e.matmul(P, lhsT=Mn[:, kb * 128:(kb + 1) * 128], rhs=bigI4[:], start=False, stop=True)
                s.op("pe", fqk, reads=[("KT", kb), ("QT", j % 3, g), mkey, "bigI4", "biasTb", "ident"], writes=pk)
                qkb[i_] = (P, pk)

            def rec_sm(i_):
                P, pk = qkb.pop(i_)
                pt = PTd[i_ % 3]
                ptk = ("PTd", i_ % 3)
                s.op("act", lambda e: e.activation(out=pt[:], in_=P, func=AF.Exp), reads=pk, writes=[ptk])

            def rec_pv(i_):
                g, kb = items[i_]
                pt = PTd[i_ % 3]
                ptk = ("PTd", i_ % 3)

                def fpv(e):
                    ins = None
                    for hh in range(4):
                        h = 4 * g + hh
                        bank, off = PObanks[h // 3], (h % 3) * 129
                        first = (kb == 0) and (h % 3 == 0)
                        ins = e.matmul(bank[:, off:off + 129], lhsT=pt[:, hh * 128:(hh + 1) * 128],
                                       rhs=V1[:, kb, g, :], start=first, stop=(kb == nkb - 1), skip_group_check=True)
                    return ins
                s.op("pe", fpv, reads=[ptk, ("V1", kb)], writes=pokeys)

            def t_item(i_):
                if i_ == 0:
                    rec_qk(0)
                if i_ + 1 < nit_:
                    rec_qk(i_ + 1)
                rec_sm(i_)
                rec_pv(i_)
            for i_ in range(nit_):
                T.append(lambda i_=i_: t_item(i_))

            def t_fin():
                for b_ in range(3):
                    nh = 3 if b_ < 2 else 2
                    s.op("act", lambda e, b_=b_, nh=nh: e.activation(
                        out=rsd[:, 3 * b_:3 * b_ + nh],
                        in_=PObanks[b_][:, 0:nh * 129].rearrange("p (h c) -> p h c", c=129)[:, :, 128],
                        func=AF.Ln), reads=pokeys, writes=[("rsd", b_)])
                s.op("act", lambda e: e.activation(out=rsd[:], in_=rsd[:], func=AF.Exp, scale=-1.0),
                     reads=[("rsd", 0), ("rsd", 1), ("rsd", 2)], writes=["rsd"])
                odt = od[j % 2]
                for h in range(8):
                    bank, off = PObanks[h // 3], (h % 3) * 129
                    s.op("act", lambda e, h=h, bank=bank, off=off: e.activation(
                        out=odt[:, h * 128:(h + 1) * 128], in_=bank[:, off:off + 128], func=AF.Copy,
                        scale=rsd[:, h:h + 1]), reads=pokeys + ["rsd"], writes=[("od", j % 2)])
                dma("od%d" % (j % 2), o_dsa_d[j * 128:(j + 1) * 128, :], odt[:], [("od", j % 2)], [("o_dsa", j)])
            T.append(t_fin)
            return T

        def run_merged(A, B):
            na, nb = len(A), len(B)
            ia = ib = 0
            while ia < na or ib < nb:
                if ib >= nb or (ia < na and ia * nb <= ib * na):
                    A[ia]()
                    ia += 1
                else:
                    B[ib]()
                    ib += 1

        def interleave(A, B):
            out_, na, nb = [], len(A), len(B)
            ia = ib = 0
            while ia < na or ib < nb:
                if ib >= nb or (ia < na and ia * nb <= ib * na):
                    out_.append(A[ia])
                    ia += 1
                else:
                    out_.append(B[ib])
                    ib += 1
            return out_

        if dsa_blocks > 0:
            run_merged(score_thunks(0), [])
        for j in range(dsa_blocks + 1):
            pa = interleave(attn_thunks(j - 1) if j >= 1 else [], score_thunks(j + 1) if j + 1 < dsa_blocks else [])
            run_merged(bis_thunks(j) if j < dsa_blocks else [], pa)
        dl.close()
        s.barrier()
        if "o_dsa" in taps:
            d = dram_out("tap_o_dsa", [dsa_blocks * 128, D], BF16)
            tapd["o_dsa"] = d
            dma("tapd", d, o_dsa_d[0:dsa_blocks * 128, :], [("o_dsa", b_) for b_ in range(dsa_blocks)], [("tapd", "o_dsa")])

    if "epi" in phases:
        el = ExitStack()

        def esb(name, shape, dt=F32):
            return el.enter_context(nc.sbuf_tensor(name, list(shape), dt))
        mergedT = esb("mergedT", [128, 8, NOWN], BF16)
        Wz = esb("Wz", [128, 8, 1024], BF16)
        Wgt = esb("Wgt", [128, 8, 1024], BF16)
        Wout = esb("Wout", [128, 8, 1024], BF16)
        yb = [esb("yb%d" % i, [128, 1024], BF16) for i in range(2)]
        sz2 = [esb("sz%d" % i, [128, 1024], BF16) for i in range(2)]
        yy = [esb("yy%d" % i, [128, 1024], BF16) for i in range(2)]
        yT = esb("yT", [128, 8, NOWN], BF16)
        sg = esb("sg", [128, 512])
        tmpm = esb("tmpm", [128, 512])
        branches = []
        if "gla" in epi_branches:
            branches.append(("gla", COL["gz"], COL["gates"], w_gla_out, o_gla_d, "o_gla"))
        if "dsa" in epi_branches:
            branches.append(("dsa", COL["dz"], COL["gates"] + 1024, w_dsa_out, o_dsa_d, "o_dsa"))
        if "mem" in epi_branches:
            branches.append(("mem", COL["xz"], COL["gates"] + 2048, w_x_out, o_mem_d, "o_mem"))
        load_w(Wz, 0, w_in, branches[0][1], 1024, "Wz", scale=gpre)
        for bi, (bname, zc, gc, wout_d, o_d, okey) in enumerate(branches):
            pre = load_w_chunks(Wgt, 0, w_in, gc, 1024, "Wgt", scale=gpre) + \
                load_w_chunks(Wout, 0, wout_d, 0, 1024, "Wout", scale=(ggl8 if bname == "gla" else None))
            zq = {}

            def zproj(blk):
                PZ, pkz = pwide()
                for half in range(2):
                    mm_group(PZ[:, half * 512:(half + 1) * 512],
                             [(hT[:, kc, blk * 128:(blk + 1) * 128], Wz[:, kc, half * 512:(half + 1) * 512])
                              for kc in range(8)], [("hT", blk), "Wz"], [pkz[half]])
                zq[blk] = (PZ, pkz)
            zproj(0)
            for blk in range(16):
                i = blk % 2
                dma("yb%d" % i, yb[i][:], o_d[blk * 128:(blk + 1) * 128, :], [(okey, blk)], [("yb", i)])
                if blk + 1 < 16:
                    zproj(blk + 1)
                PZ, pkz = zq.pop(blk)
                szt = sz2[i]
                s.op("act", lambda e, PZ=PZ, szt=szt: e.activation(out=szt[:], in_=PZ[:, :], func=AF.Silu),
                     reads=pkz, writes=[("sz", i)])
                s.op("dve", lambda e, i=i, szt=szt: e.tensor_tensor(out=yy[i][:], in0=yb[i][:], in1=szt[:], op=ALU.mult),
                     reads=[("yb", i), ("sz", i)], writes=[("yy", i)])

                def tr(e, i=i):
                    ins = None
                    for kc in range(8):
                        ins = e.transpose(out=ptr[:, kc * 128:(kc + 1) * 128], in_=yy[i][:, kc * 128:(kc + 1) * 128],
                                          identity=ident[:])
                    return ins
                s.op("pe", tr, reads=[("yy", i), "ident"], writes=["ptr"])
                s.op("dve", lambda e, blk=blk: e.tensor_copy(out=yT[:, :, blk * 128:(blk + 1) * 128],
                                                             in_=ptr[:, :].rearrange("p (k t) -> p k t", k=8)),
                     reads=["ptr"], writes=[("yT", blk)])
                if pre:
                    pre.pop(0)()
            while pre:
                pre.pop(0)()
            if bi + 1 < len(branches):
                pre = load_w_chunks(Wz, 0, w_in, branches[bi + 1][1], 1024, "Wz", scale=gpre)
            else:
                pre = load_w_chunks(Wz, 0, w_o, 0, 1024, "Wz")
            it_ = 0

            def obank():
                i = st.get("ob_i", 0) % 7
                st["ob_i"] = st.get("ob_i", 0) + 1
                if i == 6:
                    return pn[:, :], [("pb", 6)]
                return pw[i // 2][:, (i % 2) * 512:(i % 2 + 1) * 512], [("pb", i)]
            for sbk in range(4):
                tsl = slice(sbk * 512, (sbk + 1) * 512)
                hkeys = [("hT", sbk * 4 + j) for j in range(4)]
                ykeys = [("yT", sbk * 4 + j) for j in range(4)]
                for mc in range(8):
                    P1, pk1 = obank()
                    mm_group(P1, [(Wout[:, kc, mc * 128:(mc + 1) * 128], yT[:, kc, tsl]) for kc in range(8)],
                             ykeys + ["Wout"], pk1)
                    P2, pk2 = obank()
                    mm_group(P2, [(Wgt[:, kc, mc * 128:(mc + 1) * 128], hT[:, kc, tsl]) for kc in range(8)],
                             hkeys + ["Wgt"], pk2)
                    s.op("act", lambda e, P2=P2: e.activation(out=sg[:], in_=P2, func=AF.Sigmoid),
                         reads=pk2, writes=["sg"])
                    mkey = ("mT", mc, sbk)
                    if bi == 0:
                        s.op("dve", lambda e, P1=P1, mc=mc, tsl=tsl: e.tensor_tensor(
                            out=mergedT[:, mc, tsl], in0=P1, in1=sg[:], op=ALU.mult),
                            reads=pk1 + ["sg"], writes=[mkey])
                    else:
                        s.op("dve", lambda e, P1=P1: e.tensor_tensor(out=tmpm[:], in0=P1, in1=sg[:], op=ALU.mult),
                             reads=pk1 + ["sg"], writes=["tmpm"])
                        s.op("dve", lambda e, mc=mc, tsl=tsl: e.tensor_tensor(
                            out=mergedT[:, mc, tsl], in0=mergedT[:, mc, tsl], in1=tmpm[:], op=ALU.add),
                            reads=["tmpm", mkey], writes=[mkey])
                    it_ += 1
                    if pre and it_ % 4 == 0:
                        pre.pop(0)()
            while pre:
                pre.pop(0)()
        tap("mergedT", mergedT[:, :, :], [128, 8, NOWN], [("mT", mc, sbk) for mc in range(8) for sbk in range(4)], BF16)
        Wo = Wz
        gpost = esb("gpost", [128, 1024])
        load_const("c1", gpost[:], g_post_d, "gpost")
        for blk in range(16):
            sbk = blk // 4
            PF, pkf = pwide()
            for half in range(2):
                mm_group(PF[:, half * 512:(half + 1) * 512],
                         [(mergedT[:, mc, blk * 128:(blk + 1) * 128], Wo[:, mc, half * 512:(half + 1) * 512]) for mc in range(8)],
                         [("mT", mc, sbk) for mc in range(8)] + ["Wz"], [pkf[half]])
            c = stat_cols()
            s.op("act", lambda e, PF=PF, c=c: e.activation(out=junk[:], in_=PF[:, :], func=AF.Square,
                                                           accum_out=stat[:, c:c + 1]),
                 reads=pkf, writes=["junk", ("stat", c)])
            rstd_from_ss(c, 1, D)
            i = st["x_i"] % 2
            st["x_i"] += 1
            dma("x%d" % i, xin[i][:], x_own[blk * 128:(blk + 1) * 128, :], [], [("xin", i)])
            r_ = wst[blk % 2][:, 0:1024]
            s.op("dve", lambda e, PF=PF, c=c, r_=r_: e.scalar_tensor_tensor(
                out=r_, in0=PF[:, :], scalar=stat[:, c:c + 1], in1=gpost[:], op0=ALU.mult, op1=ALU.mult),
                reads=pkf + [("stat", c), "gpost"], writes=[("wst", blk % 2)])
            s.op("dve", lambda e, r_=r_, i=i: e.tensor_tensor(out=r_, in0=r_, in1=xin[i][:], op=ALU.add),
                 reads=[("wst", blk % 2), ("xin", i)], writes=[("wst", blk % 2)])
            dma("res%d" % (blk % 2), out[blk * 128:(blk + 1) * 128, :], r_, [("wst", blk % 2)], [("out", blk)])
        el.close()
        s.barrier()

    global LAST_S
    LAST_S = s
    sem_names = list(ENGS) + sorted(s.dma_sems)
    sems = {n: es.enter_context(nc.semaphore(n)) for n in sem_names}
    needed = {e: set() for e in ENGS}
    for e in ENGS:
        for (waits, fn, sem, inc) in s.ops[e]:
            for (wn, wv) in waits:
                if wn in needed:
                    needed[wn].add(wv)
    rank = {e: {v: k + 1 for k, v in enumerate(sorted(needed[e]))} for e in ENGS}
    for e in ENGS:
        cnt = 0
        new_ops = []
        for (waits, fn, sem, inc) in s.ops[e]:
            w2 = [(wn, rank[wn][wv]) if wn in rank else (wn, wv) for (wn, wv) in waits]
            if sem in rank:
                cnt += 1
                do_inc = cnt in rank[sem]
                new_ops.append((w2, fn, sem, 1 if do_inc else 0))
            else:
                new_ops.append((w2, fn, sem, inc))
        s.ops[e] = new_ops
    final_waits = [(n, s.count[n]) for n in sorted(s.dma_sems)]
    with nc.Block() as block:
        def replay(eng_name):
            def body(e):
                for (waits, fn, sem, inc) in s.ops[eng_name]:
                    for (wn, wv) in waits:
                        e.wait_ge(sems[wn], wv)
                    ins = fn(e)
                    if inc:
                        ins.then_inc(sems[sem], inc)
                if eng_name == "sp":
                    for (wn, wv) in final_waits:
                        e.wait_ge(sems[wn], wv)
            return body

        block.tensor(replay("pe"))
        block.scalar(replay("act"))
        block.vector(replay("dve"))
        block.gpsimd(replay("pool"))
        block.sync(replay("sp"))
    es.close()
    return nc, tapd


def _bf16(a):
    return np.asarray(a, dtype=np.float32).astype(ml_dtypes.bfloat16)


def _t5_bucket(dist):
    d = np.maximum(dist, 1).astype(np.float32)
    large = 16 + (np.log(d / 16) / np.log(128 / 16) * 16).astype(np.int32)
    large = np.minimum(large, 31)
    return np.where(dist < 16, dist, large)


def _consts():
    i = np.arange(128)
    same = (i[:, None] // 64) == (i[None, :] // 64)
    tribd = (same & (i[:, None] <= i[None, :])).astype(np.float32)
    trirev = (same & (i[:, None] > i[None, :])).astype(np.float32)
    return {"ident": _bf16(np.eye(128)), "tribd": _bf16(tribd), "trirev": _bf16(trirev)}


def make_in_maps(inp):
    f = lambda a: np.ascontiguousarray(np.asarray(a, dtype=np.float32))
    cst = _consts()
    shared = {
        "w_in": f(inp["w_in"][0]),
        "w_gla_out": f(inp["w_gla_out"][0]),
        "w_o": f(inp["w_o"][0]),
        "w_mem_kv": f(inp["w_mem_kv"][0]),
        "w_dsa_out": f(inp["w_dsa_out"][0]),
        "w_x_out": f(inp["w_x_out"][0]),
        "g_mem": f(np.asarray(inp["g_mem"][0]).reshape(8, 128).T),
        "g_pre": f(np.asarray(inp["g_pre"][0]).reshape(8, 128).T),
        "g_post": f(np.broadcast_to(np.asarray(inp["g_post"][0])[None, :], (128, D))),
        "g_gla": f(np.broadcast_to(np.asarray(inp["g_gla"][0])[None, :], (128, 256))),
        "g_gla8": f(np.tile(np.asarray(inp["g_gla"][0]), 4).reshape(8, 128).T),
        "w_a_up": f(inp["w_gla_a_up"][0]),
        "b_a": f(np.asarray(inp["b_gla_a"][0])[None, :]),
    }
    shared.update(cst)
    rb = np.asarray(inp["rel_bias"], dtype=np.float32)
    sl = np.arange(128)[:, None]
    tl = np.arange(128)[None, :]
    bt = np.zeros((128, 2, 8, 128), np.float32)
    for dlt in range(2):
        dist = np.maximum(128 * dlt + tl - sl, 0)
        bt[:, dlt] = rb[_t5_bucket(dist)].transpose(0, 2, 1)
    shared["biasT"] = f(bt.reshape(128, -1))
    shared["cfarT"] = f(np.broadcast_to(rb[31][None, :, None], (128, 8, 128)).reshape(128, -1))
    shared["cmask"] = f(np.where(tl.T >= sl.T, 0.0, NEG))
    shared["bigI4"] = _bf16(np.tile(np.eye(128, dtype=np.float32) * 30000.0, (1, 4)))
    shared["pow2"] = f(np.broadcast_to((2.0 ** -np.arange(32))[None, :], (128, 32)))
    maps = []
    x = np.asarray(inp["x"], dtype=np.float32)
    for c in range(8):
        b, hf = c // 2, c % 2
        m = dict(shared)
        m["x_own"] = f(x[b, hf * NOWN:(hf + 1) * NOWN])
        m["mem"] = f(inp["mem"][b])
        m["ctxb"] = np.full((128, 1), 0.0 if hf == 1 else NEG, np.float32)
        m["x_ctx"] = f(x[b, 0:NCTX]) if hf == 1 else np.zeros((NCTX, D), np.float32)
        maps.append(m)
    return maps


def kernel(**inputs):
    nc, _ = build_program()
    maps = make_in_maps(inputs)
    res = run_bass_kernel_spmd(nc, maps, core_ids=list(range(8)))
    outp = np.zeros((4, NTOK, D), np.float32)
    for c in range(8):
        b, hf = c // 2, c % 2
        outp[b, hf * NOWN:(hf + 1) * NOWN] = np.asarray(res.results[c]["out"], dtype=np.float32)
    return outp
```

```python
from contextlib import ExitStack
import os
import numpy as np
_os_environ_get = os.environ.get
import ml_dtypes
import concourse.bass as bass
import concourse.mybir as mybir
from concourse.bass_utils import run_bass_kernel_spmd

F32 = mybir.dt.float32
BF16 = mybir.dt.bfloat16
AF = mybir.ActivationFunctionType
ALU = mybir.AluOpType
AX = mybir.AxisListType

D = 1024
NOWN = 2048
NCTX = 2048
NTOK = 4096
W_IN_COLS = 11352
EPS = 1e-6
NEG = -1.0e30

_sizes = [512, 512, 1024, 16, 1024, 1024, 256, 256, 512, 64, 8, 1024, 1024, 1024, 3072]
_names = ["gq", "gk", "gv", "ga", "gz", "dq", "dk", "dv", "iq", "ik", "iw", "dz", "xq", "xz", "gates"]
COL = {}
_o = 0
for _n, _s in zip(_names, _sizes):
    COL[_n] = _o
    _o += _s
assert _o == W_IN_COLS

ENGS = ("pe", "act", "dve", "pool", "sp")


class Sch:
    def __init__(self):
        self.ops = {e: [] for e in ENGS}
        self.count = {}
        self.lastw = {}
        self.readers = {}
        self.known = {e: {} for e in ENGS}
        self.dma_sems = set()
        self.floor = {}

    def barrier(self):
        snap = dict(self.count)
        for e in ENGS:
            self.floor[e] = dict(snap)

    def op(self, eng, fn, reads=(), writes=(), dma=None):
        ps_r = [k for k in reads if k == "ptr" or (isinstance(k, tuple) and k[0] == "pb")]
        if ps_r:
            reads = [k for k in reads if k not in ps_r]
            writes = list(writes) + ps_r
        deps = {}

        def need(ev, raw):
            sem, val, src = ev
            if src is not None and src == eng and eng == "pe":
                return
            if deps.get(sem, 0) < val:
                deps[sem] = val

        for k in reads:
            w = self.lastw.get(k)
            if w is not None:
                need(w, True)
        for k in writes:
            w = self.lastw.get(k)
            if w is not None:
                need(w, False)
            for r in self.readers.get(k, {}).values():
                need(r, False)
        fl = self.floor.get(eng)
        if fl:
            for sem, val in fl.items():
                if sem != eng and deps.get(sem, 0) < val:
                    deps[sem] = val
            self.floor[eng] = None
        waits = []
        kn = self.known[eng]
        for sem, val in deps.items():
            if kn.get(sem, 0) < val:
                waits.append((sem, val))
                kn[sem] = val
        if dma is not None:
            sem = "dma_" + dma
            self.dma_sems.add(sem)
            inc = 16
            src = None
        else:
            sem = eng
            inc = 1
            src = eng
        self.count[sem] = self.count.get(sem, 0) + inc
        ev = (sem, self.count[sem], src)
        self.ops[eng].append((waits, fn, sem, inc))
        for k in writes:
            self.lastw[k] = ev
            self.readers[k] = {}
        for k in reads:
            self.readers.setdefault(k, {})[sem] = ev
        return ev


def build_program(phases=("gla", "x", "dsa", "epi"), taps=(), epi_branches=("gla", "dsa", "mem"),
                  dsa_iters=18, dsa_blocks=16, dsa_tap_blocks=()):
    nc = bass.Bass("TRN2", target_bir_lowering=False)
    s = Sch()
    es = ExitStack()
    st = {"stat_i": 0, "x_i": 0, "w_i": 0, "pb_i": 0, "pw_i": 0, "hc_i": 0}

    def dram_in(name, shape, dt=F32):
        return nc.dram_tensor(name, list(shape), dt, kind="ExternalInput").ap()

    def dram_out(name, shape, dt=F32):
        return nc.dram_tensor(name, list(shape), dt, kind="ExternalOutput").ap()

    def dram_tmp(name, shape, dt=F32):
        return nc.dram_tensor(name, list(shape), dt, kind="Internal").ap()

    def sb(name, shape, dt=F32):
        return es.enter_context(nc.sbuf_tensor(name, list(shape), dt))

    def psum(name, shape, dt=F32):
        return es.enter_context(nc.psum_tensor(name, list(shape), dt))

    x_own = dram_in("x_own", [NOWN, D])
    x_ctx = dram_in("x_ctx", [NCTX, D])
    w_in = dram_in("w_in", [D, W_IN_COLS])
    w_gla_out = dram_in("w_gla_out", [D, D])
    w_o = dram_in("w_o", [D, D])
    g_pre_d = dram_in("g_pre", [128, 8])
    g_post_d = dram_in("g_post", [128, D])
    g_gla_d = dram_in("g_gla", [128, 256])
    g_gla8_d = dram_in("g_gla8", [128, 8])
    waup_d = dram_in("w_a_up", [16, 512])
    ba_d = dram_in("b_a", [1, 512])
    ident_d = dram_in("ident", [128, 128], BF16)
    tribd_d = dram_in("tribd", [128, 128], BF16)
    trirev_d = dram_in("trirev", [128, 128], BF16)
    out = dram_out("out", [NOWN, D])
    mem_d = dram_in("mem", [256, D])
    w_mem_kv = dram_in("w_mem_kv", [D, 2048])
    w_dsa_out = dram_in("w_dsa_out", [D, D])
    w_x_out = dram_in("w_x_out", [D, D])
    g_mem_d = dram_in("g_mem", [128, 8])
    o_mem_d = dram_tmp("o_mem_scr", [NOWN, D], BF16)
    o_dsa_d = dram_tmp("o_dsa_scr", [NOWN, D], BF16)
    biasT_d = dram_in("biasT", [128, 2 * 8 * 128])
    cfarT_d = dram_in("cfarT", [128, 8 * 128])
    cmask_d = dram_in("cmask", [128, 128])
    ctxb_d = dram_in("ctxb", [128, 1])
    pow2_d = dram_in("pow2", [128, 32])
    bigI4_d = dram_in("bigI4", [128, 512], BF16)
    o_gla_d = dram_tmp("o_gla_scr", [NOWN, D], BF16)
    tapd = {}

    hT = sb("hT", [128, 8, NOWN], BF16)
    ident = sb("identsb", [128, 128], BF16)
    gpre = sb("gpre", [128, 8])
    xin = [sb("xin%d" % i, [128, D]) for i in range(2)]
    hb = [sb("hb%d" % i, [128, D], BF16) for i in range(2)]
    junk = sb("junk", [128, D], BF16)
    stat = sb("stat", [128, 64])
    wst = [sb("wst%d" % i, [128, 1024]) for i in range(4)]
    pw = [psum("pw%d" % i, [128, 1024]) for i in range(3)]
    pn = psum("pn0", [128, 512])
    ptr = psum("ptr", [128, 1024], BF16)

    def dma(ch, out_ap, in_ap, reads, writes, eng="sp"):
        s.op(eng, lambda e: e.dma_start(out=out_ap, in_=in_ap), reads=reads, writes=writes, dma=ch)

    def stat_cols(n=1):
        if st["stat_i"] % 64 + n > 64:
            st["stat_i"] += 64 - st["stat_i"] % 64
        i = st["stat_i"] % 64
        st["stat_i"] += n
        return i

    def pbank():
        i = 4 + st["pb_i"] % 3
        st["pb_i"] += 1
        if i == 6:
            return pn[:, :], [("pb", 6)]
        return pw[i // 2][:, (i % 2) * 512:(i % 2 + 1) * 512], [("pb", i)]

    def pwide():
        i = st["pw_i"] % 2
        st["pw_i"] += 1
        return pw[i], [("pb", 2 * i), ("pb", 2 * i + 1)]

    def run_merged_g(A, B):
        na, nb = len(A), len(B)
        ia = ib = 0
        while ia < na or ib < nb:
            if ib >= nb or (ia < na and ia * nb <= ib * na):
                A[ia]()
                ia += 1
            else:
                B[ib]()
                ib += 1

    def load_const(ch, dst, src, key):
        st["k_i"] = st.get("k_i", 0) + 1
        dma("k%d" % st["k_i"], dst, src, [], [key])

    load_const("c0", ident[:], ident_d, "ident")
    load_const("c1", gpre[:], g_pre_d, "gpre")
    ggl8 = sb("ggl8", [128, 8])
    load_const("c0", ggl8[:], g_gla8_d, "ggl8")

    def rstd_from_ss(c, n, dim):
        a = stat[:, c:c + n]
        keys = [("stat", c + j) for j in range(n)]
        s.op("act", lambda e: e.activation(out=a, in_=a, func=AF.Ln, scale=1.0 / dim, bias=EPS_T[:, 0:1]),
             reads=keys + ["eps"], writes=keys)
        s.op("act", lambda e: e.activation(out=a, in_=a, func=AF.Exp, scale=-0.5), reads=keys, writes=keys)

    EPS_T = sb("eps_t", [128, 2])
    s.op("pool", lambda e: e.memset(EPS_T[:, 0:1], EPS), writes=["eps"])
    s.op("pool", lambda e: e.memset(EPS_T[:, 1:2], 1.0), writes=["one"])

    def norm_block(x_dram, blk, dst, dst_slice, dst_key, split=False):
        i = st["x_i"] % 2
        st["x_i"] += 1
        xt, hbt = xin[i], hb[i]
        dma("x%d" % i, xt[:], x_dram[blk * 128:(blk + 1) * 128, :], [], [("xin", i)])
        c = stat_cols()
        ss = stat[:, c:c + 1]
        s.op("act", lambda e: e.activation(out=junk[:], in_=xt[:], func=AF.Square, accum_out=ss),
             reads=[("xin", i)], writes=["junk", ("stat", c)])
        rstd_from_ss(c, 1, D)
        s.op("dve", lambda e: e.tensor_scalar(out=hbt[:], in0=xt[:], scalar1=ss, scalar2=None, op0=ALU.mult),
             reads=[("xin", i), ("stat", c)], writes=[("hb", i)])

        def part_b():
            def tr(e):
                ins = None
                for kc in range(8):
                    ins = e.transpose(out=ptr[:, kc * 128:(kc + 1) * 128], in_=hbt[:, kc * 128:(kc + 1) * 128],
                                      identity=ident[:])
                return ins
            s.op("pe", tr, reads=[("hb", i), "ident"], writes=["ptr"])
            s.op("act", lambda e: e.activation(out=dst[:, :, dst_slice],
                                               in_=ptr[:, :].rearrange("p (k t) -> p k t", k=8), func=AF.Copy),
                 reads=["ptr"], writes=[dst_key])
        if split:
            return part_b
        part_b()

    def load_w_chunks(dst, dcol, wd, col_lo, ncols, key, scale=None, nk=8):
        T = []

        def one(kc, c0, n):
            i = st["w_i"] % 4
            st["w_i"] += 1
            dma("w%d" % i, wst[i][:, 0:n], wd[kc * 128:(kc + 1) * 128, col_lo + c0:col_lo + c0 + n],
                [], [("wst", i)])
            o_ap = dst[:, kc, dcol + c0:dcol + c0 + n]
            i_ap = wst[i][:, 0:n]
            use_act = (st["w_i"] % 2 == 0)
            if scale is not None:
                sc_ = scale[:, kc:kc + 1]
                if use_act:
                    s.op("act", lambda e: e.activation(out=o_ap, in_=i_ap, func=AF.Copy, scale=sc_),
                         reads=[("wst", i), "gpre", "gmem", "ggl8"], writes=[key])
                else:
                    s.op("dve", lambda e: e.tensor_scalar(out=o_ap, in0=i_ap, scalar1=sc_, scalar2=None, op0=ALU.mult),
                         reads=[("wst", i), "gpre", "gmem", "ggl8"], writes=[key])
            else:
                if use_act:
                    s.op("act", lambda e: e.activation(out=o_ap, in_=i_ap, func=AF.Copy), reads=[("wst", i)], writes=[key])
                else:
                    s.op("dve", lambda e: e.tensor_copy(out=o_ap, in_=i_ap), reads=[("wst", i)], writes=[key])
        for kc in range(nk):
            c0 = 0
            while c0 < ncols:
                n = min(1024, ncols - c0)
                T.append(lambda kc=kc, c0=c0, n=n: one(kc, c0, n))
                c0 += n
        return T

    def load_w(dst, dcol, wd, col_lo, ncols, key, scale=None, nk=8):
        for t_ in load_w_chunks(dst, dcol, wd, col_lo, ncols, key, scale, nk):
            t_()

    def mm_group(out_ap, pairs, reads, writes):
        def f(e):
            ins = None
            n = len(pairs)
            for j, (l, r) in enumerate(pairs):
                ins = e.matmul(out_ap, lhsT=l, rhs=r, start=(j == 0), stop=(j == n - 1))
            return ins
        s.op("pe", f, reads=reads, writes=writes)

    def tap(name, src_ap, shape, key, dt=F32):
        if name in taps:
            d = dram_out("tap_" + name, shape, dt)
            tapd[name] = d
            st["k_i"] = st.get("k_i", 0) + 1
            dma("k%d" % st["k_i"], d, src_ap, key if isinstance(key, list) else [key], [("tapd", name)])

    if "gla" not in phases:
        for blk in range(16):
            norm_block(x_own, blk, hT, slice(blk * 128, (blk + 1) * 128), ("hT", blk))
        tap("hT", hT[:, :, :], [128, 8, NOWN], ("hT", 15), BF16)

    xpre = None
    if "gla" in phases and "x" in phases:
        xw = ExitStack()
        Wkv = xw.enter_context(nc.sbuf_tensor("Wkv", [128, 8, 2048], BF16))
        Wxq = xw.enter_context(nc.sbuf_tensor("Wxq", [128, 8, 1024], BF16))
        gmem = xw.enter_context(nc.sbuf_tensor("gmem", [128, 8], F32))
        load_const("c0", gmem[:], g_mem_d, "gmem")
        xpre = load_w_chunks(Wkv, 0, w_mem_kv, 0, 2048, "Wkv", scale=gmem) + \
            load_w_chunks(Wxq, 0, w_in, COL["xq"], 1024, "Wxq", scale=gpre)
    if "gla" in phases:
        gl = ExitStack()

        def gsb(name, shape, dt=F32):
            return gl.enter_context(nc.sbuf_tensor(name, list(shape), dt))
        Wg = gsb("Wg", [128, 8, 2064], BF16)
        waup_f = gsb("waup_f", [16, 512])
        ba_f = gsb("ba_f", [1, 512])
        waup = gsb("waup", [16, 512], BF16)
        ba = gsb("ba", [1, 512], BF16)
        ones1 = gsb("ones1", [1, 128], BF16)
        tribd = gsb("tribd_sb", [128, 4, 128], BF16)
        trirev = gsb("trirev_sb", [128, 128], BF16)
        ggla = gsb("ggla", [128, 256])
        hTc = [gsb("hTc%d" % i, [128, 8, 128], BF16) for i in range(2)]
        gaT2 = [gsb("gaT%d" % i, [16, 128], BF16) for i in range(2)]
        e12 = [gsb("e1_%d" % i, [128, 512]) for i in range(2)]
        la2 = [gsb("la%d" % i, [128, 512], BF16) for i in range(2)]
        Eq2 = [gsb("Eq%d" % i, [128, 512]) for i in range(2)]
        Ek2 = [gsb("Ek%d" % i, [128, 512]) for i in range(2)]
        Er2 = [gsb("Er%d" % i, [128, 512]) for i in range(2)]
        dec2 = [gsb("dec%d" % i, [128, 4, 2]) for i in range(2)]
        qdA2 = [gsb("qdA%d" % i, [128, 4, 128], BF16) for i in range(2)]
        qdB2 = [gsb("qdB%d" % i, [128, 4, 128], BF16) for i in range(2)]
        kd2 = [gsb("kd%d" % i, [128, 4, 128], BF16) for i in range(2)]
        kt2 = [gsb("kt%d" % i, [128, 512], BF16) for i in range(2)]
        vv2 = [gsb("vv%d" % i, [128, 1024], BF16) for i in range(2)]
        attT2 = [gsb("attT%d" % i, [128, 4, 128], BF16) for i in range(2)]
        Sf = [gsb("Sf%d" % i, [128, 4, 256]) for i in range(2)]
        Sb = [gsb("Sb%d" % i, [128, 4, 256], BF16) for i in range(2)]
        og = [gsb("og%d" % i, [128, 1024], BF16) for i in range(2)]

        load_w(Wg, 0, w_in, COL["gq"], 2064, "Wg", scale=gpre)
        load_const("c0", waup_f[:], waup_d, "waup_f")
        load_const("c1", ba_f[:], ba_d, "ba_f")
        s.op("act", lambda e: e.activation(out=waup[:], in_=waup_f[:], func=AF.Copy), reads=["waup_f"], writes=["waup"])
        s.op("act", lambda e: e.activation(out=ba[:], in_=ba_f[:], func=AF.Copy), reads=["ba_f"], writes=["ba"])
        s.op("pool", lambda e: e.memset(ones1[:], 1.0), writes=["ones1"])
        for h in range(4):
            load_const("c0", tribd[:, h, :], tribd_d, ("tribd", h))
        load_const("c1", trirev[:], trirev_d, "trirev")
        load_const("c0", ggla[:], g_gla_d, "ggla")
        s.op("pool", lambda e: e.memset(Sf[0][:], 0.0), writes=[("Sf", 0)])
        s.op("pool", lambda e: e.memset(Sb[0][:], 0.0), writes=[("Sb", 0)])
        for i_ in range(2):
            s.op("pool", lambda e, i_=i_: e.memset(qdA2[i_][:], 0.0), writes=[("qdA", i_)])
            s.op("pool", lambda e, i_=i_: e.memset(qdB2[i_][:], 0.0), writes=[("qdB", i_)])
        gst = {"n": 0}
        tribd_keys = [("tribd", h) for h in range(4)]

        def gla_block(src, sl, src_key, own, oblk):
            hs = lambda kc: src[:, kc, sl]
            par = gst["n"] % 2
            gst["n"] += 1
            gaT, e1, la, Eq, Ek, Er, dec = gaT2[par], e12[par], la2[par], Eq2[par], Ek2[par], Er2[par], dec2[par]
            qdA, qdB, kd, kt, vv, attT = qdA2[par], qdB2[par], kd2[par], kt2[par], vv2[par], attT2[par]
            K_ = lambda n_: (n_, par)
            def f1():
                P, pk = pbank()
                mm_group(P[0:16, 0:128], [(Wg[:, kc, 2048:2064], hs(kc)) for kc in range(8)],
                         [src_key, "Wg"], pk)
                s.op("act", lambda e: e.activation(out=gaT[:], in_=P[0:16, 0:128], func=AF.Copy),
                     reads=pk, writes=[K_("gaT")])
            def f2():
                P2, pk2 = pbank()
                mm_group(P2, [(gaT[0:16, :], waup[0:16, :]), (ones1[0:1, :], ba[0:1, :])],
                         [K_("gaT"), "waup", "ba", "ones1"], pk2)
                s.op("act", lambda e: e.activation(out=e1[:], in_=P2, func=AF.Exp, scale=-1.0),
                     reads=pk2, writes=[K_("e1")])
                s.op("act", lambda e: e.activation(out=la[:], in_=e1[:], func=AF.Ln, bias=EPS_T[:, 1:2]),
                     reads=[K_("e1"), "one"], writes=[K_("la")])
            def f3():
                P3, pk3 = pbank()
                mm_group(P3, [(trirev[:, :], la[:, :])], ["trirev", K_("la")], pk3)
                s.op("act", lambda e: e.activation(out=Er[:], in_=P3, func=AF.Exp, scale=-1.0 / 16),
                     reads=pk3, writes=[K_("Er")])
            def f4():
                P4, pk4 = pbank()

                def fc(e):
                    ins = None
                    for h in range(4):
                        ins = e.matmul(P4[:, h * 128:(h + 1) * 128], lhsT=la[:, h * 128:(h + 1) * 128],
                                       rhs=tribd[:, 0, :], start=True, stop=True)
                    return ins
                s.op("pe", fc, reads=[K_("la")] + tribd_keys, writes=pk4)
                P4v = P4.rearrange("p (h c t) -> p h c t", h=4, c=2)
                s.op("act", lambda e: e.activation(out=dec[:], in_=P4v[:, :, :, 63], func=AF.Exp, scale=-1.0 / 16),
                     reads=pk4, writes=[K_("dec")])
                if own:
                    s.op("act", lambda e: e.activation(out=Eq[:], in_=P4, func=AF.Exp, scale=-1.0 / 16),
                         reads=pk4, writes=[K_("Eq")])
                    s.op("act", lambda e: e.activation(out=Ek[:], in_=P4, func=AF.Exp, scale=1.0 / 16),
                         reads=pk4, writes=[K_("Ek")])
            def b1():
                P5, pk5 = pbank()
                mm_group(P5, [(hs(kc), Wg[:, kc, 512:1024]) for kc in range(8)], [src_key, "Wg"], pk5)
                s.op("dve", lambda e: e.tensor_tensor(out=kt[:], in0=P5, in1=Er[:], op=ALU.mult),
                     reads=pk5 + [K_("Er")], writes=[K_("kt")])
            def b2():
                PV, pkv = pwide()
                for half in range(2):
                    mm_group(PV[:, half * 512:(half + 1) * 512],
                             [(hs(kc), Wg[:, kc, 1024 + half * 512:1024 + (half + 1) * 512]) for kc in range(8)],
                             [src_key, "Wg"], [pkv[half]])
                s.op("act", lambda e: e.activation(out=vv[:], in_=PV[:, :], func=AF.Copy), reads=pkv, writes=[K_("vv")])
            def b3():
                P6, pk6 = pbank()

                def fq(e):
                    ins = None
                    for h in range(4):
                        for kc in range(8):
                            ins = e.matmul(P6[:, h * 128:(h + 1) * 128], lhsT=Wg[:, kc, h * 128:(h + 1) * 128],
                                           rhs=hs(kc), start=(kc == 0), stop=(kc == 7))
                    return ins
                s.op("pe", fq, reads=[src_key, "Wg"], writes=pk6)
                P6v = P6.rearrange("p (h t) -> p h t", h=4)
                Eqv = Eq[:].rearrange("p (h t) -> p h t", h=4)
                s.op("dve", lambda e: e.scalar_tensor_tensor(
                    out=qdA[:, :, 0:64], in0=P6v[:, :, 0:64], scalar=128 ** -0.5, in1=Eqv[:, :, 0:64],
                    op0=ALU.mult, op1=ALU.mult), reads=pk6 + [K_("Eq")], writes=[K_("qdA")])
                s.op("dve", lambda e: e.scalar_tensor_tensor(
                    out=qdB[:, :, 64:128], in0=P6v[:, :, 64:128], scalar=128 ** -0.5, in1=Eqv[:, :, 64:128],
                    op0=ALU.mult, op1=ALU.mult), reads=pk6 + [K_("Eq")], writes=[K_("qdB")])
            def b4():
                P7, pk7 = pbank()

                def fk(e):
                    ins = None
                    for h in range(4):
                        for kc in range(8):
                            ins = e.matmul(P7[:, h * 128:(h + 1) * 128],
                                           lhsT=Wg[:, kc, 512 + h * 128:512 + (h + 1) * 128],
                                           rhs=hs(kc), start=(kc == 0), stop=(kc == 7))
                    return ins
                s.op("pe", fk, reads=[src_key, "Wg"], writes=pk7)
                s.op("dve", lambda e: e.tensor_tensor(out=kd[:].rearrange("p h t -> p (h t)"), in0=P7, in1=Ek[:],
                                                      op=ALU.mult), reads=pk7 + [K_("Ek")], writes=[K_("kd")])
            def b5():
                P8, pk8 = pbank()

                def fa(e):
                    ins = None
                    for h in range(4):
                        e.matmul(P8[:, h * 128:(h + 1) * 128], lhsT=kd[:, h, :], rhs=qdA[:, h, :],
                                 start=True, stop=False)
                        ins = e.matmul(P8[:, h * 128:(h + 1) * 128], lhsT=kd[:, h, :], rhs=qdB[:, h, :],
                                       start=False, stop=True)
                    return ins
                s.op("pe", fa, reads=[K_("kd"), K_("qdA"), K_("qdB")], writes=pk8)
                s.op("dve", lambda e: e.tensor_tensor(out=attT[:].rearrange("p h t -> p (h t)"), in0=P8,
                                                      in1=tribd[:].rearrange("p h t -> p (h t)"), op=ALU.mult),
                     reads=pk8 + tribd_keys, writes=[K_("attT")])

            def state_update(c, s_in, s_out):
                PU, pku = pwide()

                def fu(e):
                    ins = None
                    for h in range(4):
                        ins = e.matmul(PU[:, h * 256:(h + 1) * 256], lhsT=kt[c * 64:(c + 1) * 64, h * 128:(h + 1) * 128],
                                       rhs=vv[c * 64:(c + 1) * 64, h * 256:(h + 1) * 256], start=True, stop=True)
                    return ins
                s.op("pe", fu, reads=[K_("kt"), K_("vv")], writes=pku)
                for h in range(4):
                    s.op("dve", lambda e, h=h: e.scalar_tensor_tensor(
                        out=Sf[s_out][:, h, :], in0=Sf[s_in][:, h, :], scalar=dec[:, h, c:c + 1],
                        in1=PU[:, h * 256:(h + 1) * 256], op0=ALU.mult, op1=ALU.add),
                        reads=pku + [("Sf", s_in), K_("dec")], writes=[("Sf", s_out)])
                s.op("dve", lambda e: e.tensor_copy(out=Sb[s_out][:], in_=Sf[s_out][:]),
                     reads=[("Sf", s_out)], writes=[("Sb", s_out)])

            def b7():
                PO, pko = pwide()

                def fo(e):
                    ins = None
                    for h in range(4):
                        o_ap = PO[:, h * 256:(h + 1) * 256]
                        e.matmul(o_ap, lhsT=attT[:, h, :], rhs=vv[:, h * 256:(h + 1) * 256], start=True, stop=False)
                        e.matmul(o_ap, lhsT=qdA[:, h, :], rhs=Sb[0][:, h, :], start=False, stop=False)
                        ins = e.matmul(o_ap, lhsT=qdB[:, h, :], rhs=Sb[1][:, h, :], start=False, stop=True)
                    return ins
                s.op("pe", fo, reads=[K_("attT"), K_("vv"), K_("qdA"), K_("qdB"), ("Sb", 0), ("Sb", 1)], writes=pko)
                c = stat_cols(4)
                for h in range(4):
                    s.op("act", lambda e, h=h: e.activation(out=junk[:, h * 256:(h + 1) * 256], in_=PO[:, h * 256:(h + 1) * 256],
                                                            func=AF.Square, accum_out=stat[:, c + h:c + h + 1]),
                         reads=pko, writes=["junk", ("stat", c + h)])
                rstd_from_ss(c, 4, 256)
                ogt = og[oblk % 2]
                for h in range(4):
                    s.op("act", lambda e, h=h: e.activation(
                        out=ogt[:, h * 256:(h + 1) * 256], in_=PO[:, h * 256:(h + 1) * 256], func=AF.Copy,
                        scale=stat[:, c + h:c + h + 1]),
                        reads=pko + [("stat", c + h)], writes=[("og", oblk % 2)])
                dma("og%d" % (oblk % 2), o_gla_d[oblk * 128:(oblk + 1) * 128, :], ogt[:],
                    [("og", oblk % 2)], [("o_gla", oblk)], eng="pool")
            front = [f1, f2, f3, f4]
            back = [b1, b2]
            if own:
                back += [b3, b4, b5]
            back.append(lambda: state_update(0, 0, 1))
            if own:
                back.append(b7)
            back.append(lambda: state_update(1, 1, 0))
            return front, back

        prev_back = []
        for blk in range(32):
            if blk < 16:
                i = st["hc_i"] % 2
                st["hc_i"] += 1
                norm_block(x_ctx, blk, hTc[i], slice(0, 128), ("hTc", i))
                norm_block(x_own, blk, hT, slice(blk * 128, (blk + 1) * 128), ("hT", blk))
                front, back = gla_block(hTc[i], slice(0, 128), ("hTc", i), False, None)
            else:
                ob = blk - 16
                front, back = gla_block(hT, slice(ob * 128, (ob + 1) * 128), ("hT", ob), True, ob)
            run_merged_g(prev_back, front)
            prev_back = back
            if xpre and blk >= 6:
                xpre.pop(0)()
        run_merged_g(prev_back, [])
        if "mem" in _os_environ_get("DBG", ""):
            print("GLA sbuf remaining", nc.sbuf_bytes_remaining)
        gl.close()
        s.barrier()
        if "o_gla" in taps:
            d = dram_out("tap_o_gla", [NOWN, D], BF16)
            tapd["o_gla"] = d
            dma("tapg", d, o_gla_d, [("o_gla", b_) for b_ in range(16)], [("tapd", "o_gla")])


    if "x" in phases:
        xl = ExitStack()

        def xsb(name, shape, dt=F32):
            return xl.enter_context(nc.sbuf_tensor(name, list(shape), dt))
        if xpre is None:
            Wkv = xsb("Wkv", [128, 8, 2048], BF16)
            Wxq = xsb("Wxq", [128, 8, 1024], BF16)
            gmem = xsb("gmem", [128, 8])
        memT = xsb("memT", [128, 8, 256], BF16)
        mkT = xsb("mkT", [128, 8, 256], BF16)
        mv1 = xsb("mv1", [128, 2, 4, 256], BF16)
        onesc = xsb("onesc", [128, 2], BF16)
        xqT = xsb("xqT", [128, 8, 512], BF16)
        PT = [[xsb("PT%d_%d" % (h, m), [128, 512], BF16) for m in range(2)] for h in range(4)]
        rs = xsb("rsx", [128, 4])
        om = [xsb("om%d" % i, [128, 1024], BF16) for i in range(2)]
        s.op("pool", lambda e: e.memset(onesc[:], 1.0), writes=["onesc"])
        if xpre is None:
            load_const("c0", gmem[:], g_mem_d, "gmem")
            load_w(Wkv, 0, w_mem_kv, 0, 2048, "Wkv", scale=gmem)
            load_w(Wxq, 0, w_in, COL["xq"], 1024, "Wxq", scale=gpre)
        else:
            while xpre:
                xpre.pop(0)()
        for mb in range(2):
            norm_block(mem_d, mb, memT, slice(mb * 128, (mb + 1) * 128), ("memT", mb))
        memk = [("memT", 0), ("memT", 1)]
        for hd in range(8):
            P, pk = pbank()
            mm_group(P[:, 0:256], [(Wkv[:, kc, hd * 128:(hd + 1) * 128], memT[:, kc, :]) for kc in range(8)],
                     memk + ["Wkv"], pk)
            s.op("act", lambda e, P=P, hd=hd: e.activation(out=mkT[:, hd, :], in_=P[:, 0:256], func=AF.Copy),
                 reads=pk, writes=["mkT"])
        for mb in range(2):
            for half in range(2):
                P, pk = pbank()
                mm_group(P, [(memT[:, kc, mb * 128:(mb + 1) * 128],
                              Wkv[:, kc, 1024 + half * 512:1024 + (half + 1) * 512]) for kc in range(8)],
                         memk + ["Wkv"], pk)
                s.op("act", lambda e, P=P, mb=mb, half=half: e.activation(
                    out=mv1[:, mb, 2 * half:2 * half + 2, :].rearrange("p h d -> p (h d)"), in_=P, func=AF.Copy),
                    reads=pk, writes=["mv1"])
        for sbk in range(4):
            tsl = slice(sbk * 512, (sbk + 1) * 512)
            hkeys = [("hT", sbk * 4 + j) for j in range(4)]
            for hd in range(8):
                P, pk = pbank()
                mm_group(P, [(Wxq[:, kc, hd * 128:(hd + 1) * 128], hT[:, kc, tsl]) for kc in range(8)],
                         hkeys + ["Wxq"], pk)
                s.op("act", lambda e, P=P, hd=hd: e.activation(out=xqT[:, hd, :], in_=P, func=AF.Copy, scale=1.0 / 16),
                     reads=pk, writes=[("xqT", hd)])
            for h in range(4):
                for mb in range(2):
                    P, pk = pbank()
                    mm_group(P, [(mkT[:, 2 * h + dc, mb * 128:(mb + 1) * 128], xqT[:, 2 * h + dc, :]) for dc in range(2)],
                             ["mkT", ("xqT", 2 * h), ("xqT", 2 * h + 1)], pk)
                    s.op("act", lambda e, P=P, h=h, mb=mb: e.activation(out=PT[h][mb][:], in_=P, func=AF.Exp),
                         reads=pk, writes=[("PT", h, mb)])
            for j in range(4):
                blk = sbk * 4 + j
                PO, pko = pwide()
                PS, pks = pbank()

                def fx(e, PO=PO, PS=PS, j=j):
                    ins = None
                    for h in range(4):
                        for mb in range(2):
                            e.matmul(PO[:, h * 256:(h + 1) * 256], lhsT=PT[h][mb][:, j * 128:(j + 1) * 128],
                                     rhs=mv1[:, mb, h, :], start=(mb == 0), stop=(mb == 1))
                    for h in range(4):
                        for mb in range(2):
                            ins = e.matmul(PS[:, 2 * h:2 * h + 2], lhsT=PT[h][mb][:, j * 128:(j + 1) * 128],
                                           rhs=onesc[:, 0:2], start=(mb == 0), stop=(mb == 1))
                    return ins
                s.op("pe", fx, reads=[("PT", h, mb) for h in range(4) for mb in range(2)] + ["mv1", "onesc"],
                     writes=pko + pks)
                s.op("dve", lambda e, PS=PS: e.reciprocal(out=rs[:], in_=PS[:, 0:8].rearrange("p (h two) -> p h two", two=2)[:, :, 0]),
                     reads=pks, writes=["rsx"])
                omt = om[blk % 2]
                for h in range(4):
                    s.op("act", lambda e, h=h, PO=PO, omt=omt: e.activation(
                        out=omt[:, h * 256:(h + 1) * 256], in_=PO[:, h * 256:(h + 1) * 256], func=AF.Copy,
                        scale=rs[:, h:h + 1]), reads=pko + ["rsx"], writes=[("om", blk % 2)])
                dma("om%d" % (blk % 2), o_mem_d[blk * 128:(blk + 1) * 128, :], omt[:],
                    [("om", blk % 2)], [("o_mem", blk)], eng="pool")
        xl.close()
        if "gla" in phases:
            xw.close()
        s.barrier()
        if "o_mem" in taps:
            d = dram_out("tap_o_mem", [NOWN, D], BF16)
            tapd["o_mem"] = d
            dma("tapm", d, o_mem_d, [("o_mem", b_) for b_ in range(16)], [("tapd", "o_mem")])


    if "dsa" in phases:
        NIT = dsa_iters
        dl = ExitStack()

        def dsb(name, shape, dt=F32):
            return dl.enter_context(nc.sbuf_tensor(name, list(shape), dt))
        KT = dsb("KT", [128, 2, NTOK], BF16)
        V1 = dsb("V1", [128, 32, 2, 129], BF16)
        ikT2 = dsb("ikT2", [128, NTOK], BF16)
        biasTb = dsb("biasTb", [128, 2, 8, 128], BF16)
        cmask = dsb("cmask_sb", [128, 128])
        ctxb = dsb("ctxb_sb", [128, 1])
        pow2 = dsb("pow2_sb", [128, 32])
        onesd = dsb("onesd", [128, 2], BF16)
        bigI4 = dsb("bigI4_sb", [128, 512], BF16)
        hTc2 = [dsb("hTd%d" % i, [128, 8, 128], BF16) for i in range(2)]
        scb = [dsb("sc%d" % i, [128, NTOK]) for i in range(2)]
        bst = scb[0][:, 0:2048].rearrange("p (a h t) -> p a h t", a=2, h=8)
        cst_ = scb[0][:, 2048:3072].rearrange("p (h t) -> p h t", h=8)
        load_const("c0", scb[0][:, 0:2048], biasT_d, "bst")
        load_const("c1", scb[0][:, 2048:3072], cfarT_d, "cst")
        load_const("c0", cmask[:], cmask_d, "cmask")
        load_const("c1", ctxb[:], ctxb_d, "ctxb")
        load_const("c0", pow2[:], pow2_d, "pow2")
        load_const("c1", bigI4[:], bigI4_d, "bigI4")
        s.op("pool", lambda e: e.memset(onesd[:], 1.0), writes=["onesd"])
        s.op("pool", lambda e: e.memset(V1[:, :, :, 128:129], 1.0), writes=["V1ones"])
        for a in range(2):
            s.op("dve", lambda e, a=a: e.tensor_tensor(out=biasTb[:, a], in0=bst[:, a], in1=cst_, op=ALU.subtract),
                 reads=["bst", "cst"], writes=["biasTb", ("sc", 0)])
        dst = {"i": 0}

        def dbank():
            i = 3 + dst["i"] % 4
            dst["i"] += 1
            if i == 6:
                return pn[:, :], [("pb", 6)]
            return pw[i // 2][:, (i % 2) * 512:(i % 2 + 1) * 512], [("pb", i)]

        kl = ExitStack()
        Wkv2 = kl.enter_context(nc.sbuf_tensor("Wkv2", [128, 8, 640], BF16))
        load_w(Wkv2, 0, w_in, COL["dk"], 512, "Wkv2", scale=gpre)
        load_w(Wkv2, 512, w_in, COL["ik"], 64, "Wkv2", scale=gpre)
        load_w(Wkv2, 576, w_in, COL["ik"], 64, "Wkv2", scale=gpre)
        norm_block(x_ctx, 0, hTc2[0], slice(0, 128), ("hTd", 0))
        for kb in range(32):
            nb_late = None
            if kb < 16:
                i = kb % 2
                if kb + 1 < 16:
                    nb_late = norm_block(x_ctx, kb + 1, hTc2[(kb + 1) % 2], slice(0, 128), ("hTd", (kb + 1) % 2), split=True)
                src_, sl_, skey = hTc2[i], slice(0, 128), ("hTd", i)
            else:
                src_, sl_, skey = hT, slice((kb - 16) * 128, (kb - 15) * 128), ("hT", kb - 16)
            P, pk = dbank()

            def fkk(e, P=P, src_=src_, sl_=sl_):
                ins = None
                for g in range(2):
                    for kc in range(8):
                        ins = e.matmul(P[:, g * 128:(g + 1) * 128], lhsT=Wkv2[:, kc, g * 128:(g + 1) * 128],
                                       rhs=src_[:, kc, sl_], start=(kc == 0), stop=(kc == 7))
                for kc in range(8):
                    ins = e.matmul(P[:, 256:384], lhsT=Wkv2[:, kc, 512:640], rhs=src_[:, kc, sl_],
                                   start=(kc == 0), stop=(kc == 7))
                return ins
            s.op("pe", fkk, reads=[skey, "Wkv2"], writes=pk)
            s.op("act", lambda e, P=P, kb=kb: e.activation(
                out=KT[:, :, kb * 128:(kb + 1) * 128], in_=P[:, 0:256].rearrange("p (g t) -> p g t", g=2), func=AF.Copy),
                reads=pk, writes=[("KT", kb)])
            s.op("dve", lambda e, P=P, kb=kb: e.tensor_copy(out=ikT2[:, kb * 128:(kb + 1) * 128], in_=P[:, 256:384]),
                 reads=pk, writes=[("ikT", kb)])
            P2, pk2 = dbank()
            mm_group(P2[:, 0:256], [(src_[:, kc, sl_], Wkv2[:, kc, 256:512]) for kc in range(8)], [skey, "Wkv2"], pk2)
            s.op("act", lambda e, P2=P2, kb=kb: e.activation(out=V1[:, kb, :, 0:128],
                                                            in_=P2[:, 0:256].rearrange("p (g d) -> p g d", g=2), func=AF.Copy),
                 reads=pk2 + ["V1ones"], writes=[("V1", kb)])
            if nb_late is not None:
                nb_late()
        kl.close()
        s.barrier()
        if "KT" in taps:
            tap("KT", KT[:, :, :], [128, 2, NTOK], [("KT", kb) for kb in range(32)], BF16)
            tap("V1", V1[:, :, :, :], [128, 32, 2, 129], [("V1", kb) for kb in range(32)], BF16)
            tap("ikT2", ikT2[:, :], [128, NTOK], [("ikT", kb) for kb in range(32)], BF16)

        Wq = dsb("Wq", [128, 8, 1544], BF16)
        load_w(Wq, 0, w_in, COL["dq"], 1024, "Wq", scale=gpre)
        load_w(Wq, 1024, w_in, COL["iq"], 512, "Wq", scale=gpre)
        load_w(Wq, 1536, w_in, COL["iw"], 8, "Wq", scale=gpre)
        QTb = [dsb("QT%d" % i, [128, 8, 128], BF16) for i in range(3)]
        iqT2 = dsb("iqT2", [128, 4, 128], BF16)
        iwb = dsb("iwb", [128, 8])
        Dg = dsb("Dg", [128, 8, 128], BF16)
        rl = [dsb("rl%d" % i, [128, 512], BF16) for i in range(3)]
        Mnb = [dsb("Mn%d" % i, [128, NTOK], BF16) for i in range(2)]
        PTd = [dsb("PTd%d" % i, [128, 512], BF16) for i in range(3)]
        bs = dsb("bis", [128, 40])
        rsd = dsb("rsd", [128, 8])
        od = [dsb("od%d" % i, [128, 1024], BF16) for i in range(2)]
        ISC = (8 ** -0.5) * (64 ** -0.5)
        if "mem" in _os_environ_get("DBG", ""):
            print("DSA sbuf remaining", nc.sbuf_bytes_remaining)
        PACC = ptr[:, :].bitcast(F32)
        sst = {"s": 0, "a": 0, "r": 0}

        def sbank():
            i = 5 + sst["s"] % 2
            sst["s"] += 1
            if i == 6:
                return pn[:, :], [("pb", 6)]
            return pw[2][:, 512:1024], [("pb", 5)]

        def abank():
            i = 3 + sst["a"] % 2
            sst["a"] += 1
            if i == 3:
                return pw[1][:, 512:1024], [("pb", 3)]
            return pw[2][:, 0:512], [("pb", 4)]

        def score_thunks(j):
            T = []
            hsl = slice(j * 128, (j + 1) * 128)
            hk = ("hT", j)
            nkb = 17 + j
            N = nkb * 128
            QTj = QTb[j % 3]
            scj = scb[j % 2]

            def t_q(g):
                P, pk = sbank()

                def fq(e):
                    ins = None
                    for hh in range(4):
                        h = 4 * g + hh
                        for kc in range(8):
                            ins = e.matmul(P[:, hh * 128:(hh + 1) * 128], lhsT=Wq[:, kc, h * 128:(h + 1) * 128],
                                           rhs=hT[:, kc, hsl], start=(kc == 0), stop=(kc == 7))
                    return ins
                s.op("pe", fq, reads=[hk, "Wq"], writes=pk)
                s.op("act", lambda e: e.activation(
                    out=QTj[:, 4 * g:4 * g + 4, :].rearrange("p h t -> p (h t)"), in_=P, func=AF.Copy, scale=128 ** -0.5),
                    reads=pk, writes=[("QT", j % 3, g)])
            T.append(lambda: t_q(0))
            T.append(lambda: t_q(1))

            def t_iq():
                P, pk = sbank()

                def fiq(e):
                    ins = None
                    for c in range(4):
                        for kc in range(8):
                            ins = e.matmul(P[:, c * 128:(c + 1) * 128], lhsT=Wq[:, kc, 1024 + c * 128:1024 + (c + 1) * 128],
                                           rhs=hT[:, kc, hsl], start=(kc == 0), stop=(kc == 7))
                    return ins
                s.op("pe", fiq, reads=[hk, "Wq"], writes=pk)
                s.op("act", lambda e: e.activation(out=iqT2[:].rearrange("p c t -> p (c t)"), in_=P, func=AF.Copy),
                     reads=pk, writes=["iqT2"])
                P2, pk2 = sbank()
                mm_group(P2[:, 0:8], [(hT[:, kc, hsl], Wq[:, kc, 1536:1544]) for kc in range(8)], [hk, "Wq"], pk2)
                s.op("act", lambda e: e.activation(out=iwb[:], in_=P2[:, 0:8], func=AF.Copy, scale=ISC),
                     reads=pk2, writes=["iwb"])
                for h in range(8):
                    s.op("act", lambda e, h=h: e.activation(out=Dg[:, h, :], in_=ident[:], func=AF.Copy,
                                                            scale=iwb[:, h:h + 1]),
                         reads=["ident", "iwb"], writes=[("Dg", h)])
            T.append(t_iq)
            nch = (N + 511) // 512
            sckeys = [("sc", j % 2, i_) for i_ in range(nch)]
            pend = {}

            def rec_s(ci, h):
                c0 = ci * 512
                n = min(512, N - c0)
                kkeys = [("ikT", kb) for kb in range(c0 // 128, (c0 + n) // 128)]
                P, pk = sbank()
                pr = (h % 2) * 64
                s.op("pe", lambda e: e.matmul(P[:, 0:n], lhsT=iqT2[pr:pr + 64, h // 2, :], rhs=ikT2[pr:pr + 64, c0:c0 + n],
                                              start=True, stop=True), reads=["iqT2"] + kkeys, writes=pk)
                pend[(ci, h)] = (P, pk)

            def rec_acc(ci, h):
                c0 = ci * 512
                n = min(512, N - c0)
                P, pk = pend.pop((ci, h))
                ri = sst["r"] % 3
                sst["r"] += 1
                r_ = rl[ri]
                s.op("act", lambda e: e.activation(out=r_[:, 0:n], in_=P[:, 0:n], func=AF.Relu),
                     reads=pk, writes=[("rl", ri)])
                s.op("pe", lambda e: e.matmul(PACC[:, 0:n], lhsT=Dg[:, h, :], rhs=r_[:, 0:n], start=(h == 0), stop=(h == 7),
                                              skip_group_check=True),
                     reads=[("rl", ri), ("Dg", h)], writes=["ptr"])
                if h == 7:
                    s.op("act", lambda e: e.activation(out=scj[:, c0:c0 + n], in_=PACC[:, 0:n], func=AF.Copy),
                         reads=["ptr"], writes=[("sc", j % 2, ci)])
            seq = [(ci, h) for ci in range(nch) for h in range(8)]

            def t_step(k_):
                if k_ == 0:
                    rec_s(*seq[0])
                if k_ + 1 < len(seq):
                    rec_s(*seq[k_ + 1])
                rec_acc(*seq[k_])
            for k_ in range(len(seq)):
                T.append(lambda k_=k_: t_step(k_))
            return T

        def bis_thunks(j):
            T = []
            nkb = 17 + j
            N = nkb * 128
            scj = scb[j % 2]
            Mn = Mnb[j % 2]
            nch = (N + 511) // 512
            sckeys = [("sc", j % 2, i_) for i_ in range(nch)]
            mkey = ("Mn", j % 2)

            def t_prep():
                s.op("dve", lambda e: e.tensor_reduce(out=bs[:, 0:1], in_=scj[:, 0:N], axis=AX.X, op=ALU.max,
                                                      apply_absolute_value=True), reads=sckeys, writes=["bs_R"])
                s.op("dve", lambda e: e.tensor_scalar(out=bs[:, 0:1], in0=bs[:, 0:1], scalar1=1.01, scalar2=1e-6,
                                                      op0=ALU.mult, op1=ALU.add), reads=["bs_R"], writes=["bs_R"])
                s.op("dve", lambda e: e.tensor_scalar(out=bs[:, 8:8 + NIT + 1], in0=pow2[:, 0:NIT + 1], scalar1=bs[:, 0:1],
                                                      scalar2=None, op0=ALU.mult), reads=["bs_R", "pow2"], writes=["bs_w"])
                s.op("dve", lambda e: e.tensor_scalar(out=scj[:, 0:NCTX], in0=scj[:, 0:NCTX], scalar1=ctxb[:, 0:1],
                                                      scalar2=None, op0=ALU.add),
                     reads=sckeys + ["ctxb", "bs_R"], writes=sckeys)
                s.op("dve", lambda e: e.tensor_tensor(out=scj[:, N - 128:N], in0=scj[:, N - 128:N], in1=cmask[:], op=ALU.add),
                     reads=sckeys + ["cmask", "bs_R"], writes=sckeys)
                s.op("dve", lambda e: e.memset(bs[:, 1:2], 0.0), writes=["bs_mid"])
            T.append(t_prep)

            def t_bis(it):
                s.op("dve", lambda e: e.tensor_scalar(out=Mn[:, 0:N], in0=scj[:, 0:N], scalar1=bs[:, 1:2], scalar2=None,
                                                      op0=ALU.is_ge, op1=ALU.add, accum_out=bs[:, 2:3]),
                     reads=sckeys + ["bs_mid"], writes=[mkey, "bs_cnt"])
                s.op("dve", lambda e: e.tensor_scalar(out=bs[:, 3:4], in0=bs[:, 2:3], scalar1=255.5,
                                                      scalar2=bs[:, 8 + it:9 + it], op0=ALU.is_ge, op1=ALU.mult),
                     reads=["bs_cnt", "bs_w"], writes=["bs_g"])
                s.op("dve", lambda e: e.scalar_tensor_tensor(out=bs[:, 1:2], in0=bs[:, 3:4],
                                                             scalar=bs[:, 9 + it:10 + it], in1=bs[:, 1:2],
                                                             op0=ALU.subtract, op1=ALU.add),
                     reads=["bs_g", "bs_w", "bs_mid"], writes=["bs_mid"])
            for it in range(NIT):
                T.append(lambda it=it: t_bis(it))

            def t_mask():
                s.op("dve", lambda e: e.tensor_tensor(out=bs[:, 4:5], in0=bs[:, 1:2], in1=bs[:, 8 + NIT:9 + NIT],
                                                      op=ALU.subtract), reads=["bs_mid", "bs_w"], writes=["bs_lo"])
                s.op("dve", lambda e: e.tensor_scalar(out=Mn[:, 0:N], in0=scj[:, 0:N], scalar1=bs[:, 4:5], scalar2=1.0,
                                                      op0=ALU.is_ge, op1=ALU.subtract), reads=sckeys + ["bs_lo"],
                     writes=[mkey])
                if j in dsa_tap_blocks:
                    tap("sc%d" % j, scj[:, 0:N], [128, N], sckeys)
                    tap("Mm%d" % j, Mn[:, 0:N], [128, N], [mkey], BF16)
            T.append(t_mask)
            return T

        PObanks = [pw[0][:, 0:512], pw[0][:, 512:1024], pw[1][:, 0:512]]
        pokeys = [("pb", 0), ("pb", 1), ("pb", 2)]

        def attn_thunks(j):
            T = []
            nkb = 17 + j
            QTj = QTb[j % 3]
            Mn = Mnb[j % 2]
            mkey = ("Mn", j % 2)
            items = [(g, kb) for g in range(2) for kb in range(nkb)]
            nit_ = len(items)
            qkb = {}

            def rec_qk(i_):
                g, kb = items[i_]
                delta = 16 + j - kb
                P, pk = abank()

                def fqk(e):
                    e.matmul(P, lhsT=KT[:, g, kb * 128:(kb + 1) * 128],
                             rhs=QTj[:, 4 * g:4 * g + 4, :].rearrange("p h t -> p (h t)"), start=True, stop=False)
                    if delta < 2:
                        e.matmul(P, lhsT=ident[:], rhs=biasTb[:, delta, 4 * g:4 * g + 4, :].rearrange("p h t -> p (h t)"),
                                 start=False, stop=False)
                    return e.matmul(P, lhsT=Mn[:, kb * 128:(kb + 1) * 128], rhs=bigI4[:], start=False, stop=True)
                s.op("pe", fqk, reads=[("KT", kb), ("QT", j % 3, g), mkey, "bigI4", "biasTb", "ident"], writes=pk)
                qkb[i_] = (P, pk)

            def rec_sm(i_):
                P, pk = qkb.pop(i_)
                pt = PTd[i_ % 3]
                ptk = ("PTd", i_ % 3)
                s.op("act", lambda e: e.activation(out=pt[:], in_=P, func=AF.Exp), reads=pk, writes=[ptk])

            def rec_pv(i_):
                g, kb = items[i_]
                pt = PTd[i_ % 3]
                ptk = ("PTd", i_ % 3)

                def fpv(e):
                    ins = None
                    for hh in range(4):
                        h = 4 * g + hh
                        bank, off = PObanks[h // 3], (h % 3) * 129
                        first = (kb == 0) and (h % 3 == 0)
                        ins = e.matmul(bank[:, off:off + 129], lhsT=pt[:, hh * 128:(hh + 1) * 128],
                                       rhs=V1[:, kb, g, :], start=first, stop=(kb == nkb - 1), skip_group_check=True)
                    return ins
                s.op("pe", fpv, reads=[ptk, ("V1", kb)], writes=pokeys)

            def t_item(i_):
                if i_ == 0:
                    rec_qk(0)
                if i_ + 1 < nit_:
                    rec_qk(i_ + 1)
                rec_sm(i_)
                rec_pv(i_)
            for i_ in range(nit_):
                T.append(lambda i_=i_: t_item(i_))

            def t_fin():
                for b_ in range(3):
                    nh = 3 if b_ < 2 else 2
                    s.op("act", lambda e, b_=b_, nh=nh: e.activation(
                        out=rsd[:, 3 * b_:3 * b_ + nh],
                        in_=PObanks[b_][:, 0:nh * 129].rearrange("p (h c) -> p h c", c=129)[:, :, 128],
                        func=AF.Ln), reads=pokeys, writes=[("rsd", b_)])
                s.op("act", lambda e: e.activation(out=rsd[:], in_=rsd[:], func=AF.Exp, scale=-1.0),
                     reads=[("rsd", 0), ("rsd", 1), ("rsd", 2)], writes=["rsd"])
                odt = od[j % 2]
                for h in range(8):
                    bank, off = PObanks[h // 3], (h % 3) * 129
                    s.op("act", lambda e, h=h, bank=bank, off=off: e.activation(
                        out=odt[:, h * 128:(h + 1) * 128], in_=bank[:, off:off + 128], func=AF.Copy,
                        scale=rsd[:, h:h + 1]), reads=pokeys + ["rsd"], writes=[("od", j % 2)])
                dma("od%d" % (j % 2), o_dsa_d[j * 128:(j + 1) * 128, :], odt[:], [("od", j % 2)], [("o_dsa", j)], eng="pool")
            T.append(t_fin)
            return T

        def run_merged(A, B):
            na, nb = len(A), len(B)
            ia = ib = 0
            while ia < na or ib < nb:
                if ib >= nb or (ia < na and ia * nb <= ib * na):
                    A[ia]()
                    ia += 1
                else:
                    B[ib]()
                    ib += 1

        def interleave(A, B):
            out_, na, nb = [], len(A), len(B)
            ia = ib = 0
            while ia < na or ib < nb:
                if ib >= nb or (ia < na and ia * nb <= ib * na):
                    out_.append(A[ia])
                    ia += 1
                else:
                    out_.append(B[ib])
                    ib += 1
            return out_

        if dsa_blocks > 0:
            run_merged(score_thunks(0), [])
        for j in range(dsa_blocks + 1):
            pa = interleave(attn_thunks(j - 1) if j >= 1 else [], score_thunks(j + 1) if j + 1 < dsa_blocks else [])
            run_merged(bis_thunks(j) if j < dsa_blocks else [], pa)
        dl.close()
        s.barrier()
        if "o_dsa" in taps:
            d = dram_out("tap_o_dsa", [dsa_blocks * 128, D], BF16)
            tapd["o_dsa"] = d
            dma("tapd", d, o_dsa_d[0:dsa_blocks * 128, :], [("o_dsa", b_) for b_ in range(dsa_blocks)], [("tapd", "o_dsa")])

    if "epi" in phases:
        el = ExitStack()

        def esb(name, shape, dt=F32):
            return el.enter_context(nc.sbuf_tensor(name, list(shape), dt))
        mergedT = esb("mergedT", [128, 8, NOWN], BF16)
        Wz = esb("Wz", [128, 8, 1024], BF16)
        Wgt = esb("Wgt", [128, 8, 1024], BF16)
        Wout = esb("Wout", [128, 8, 1024], BF16)
        yb = [esb("yb%d" % i, [128, 1024], BF16) for i in range(2)]
        sz2 = [esb("sz%d" % i, [128, 1024], BF16) for i in range(2)]
        yy = [esb("yy%d" % i, [128, 1024], BF16) for i in range(2)]
        yT = esb("yT", [128, 8, NOWN], BF16)
        sg = esb("sg", [128, 512])
        tmpm = esb("tmpm", [128, 512])
        branches = []
        if "gla" in epi_branches:
            branches.append(("gla", COL["gz"], COL["gates"], w_gla_out, o_gla_d, "o_gla"))
        if "dsa" in epi_branches:
            branches.append(("dsa", COL["dz"], COL["gates"] + 1024, w_dsa_out, o_dsa_d, "o_dsa"))
        if "mem" in epi_branches:
            branches.append(("mem", COL["xz"], COL["gates"] + 2048, w_x_out, o_mem_d, "o_mem"))
        load_w(Wz, 0, w_in, branches[0][1], 1024, "Wz", scale=gpre)
        for bi, (bname, zc, gc, wout_d, o_d, okey) in enumerate(branches):
            pre = load_w_chunks(Wgt, 0, w_in, gc, 1024, "Wgt", scale=gpre) + \
                load_w_chunks(Wout, 0, wout_d, 0, 1024, "Wout", scale=(ggl8 if bname == "gla" else None))
            zq = {}

            def zproj(blk):
                PZ, pkz = pwide()
                for half in range(2):
                    mm_group(PZ[:, half * 512:(half + 1) * 512],
                             [(hT[:, kc, blk * 128:(blk + 1) * 128], Wz[:, kc, half * 512:(half + 1) * 512])
                              for kc in range(8)], [("hT", blk), "Wz"], [pkz[half]])
                zq[blk] = (PZ, pkz)
            zproj(0)
            for blk in range(16):
                i = blk % 2
                dma("yb%d" % i, yb[i][:], o_d[blk * 128:(blk + 1) * 128, :], [(okey, blk)], [("yb", i)])
                if blk + 1 < 16:
                    zproj(blk + 1)
                PZ, pkz = zq.pop(blk)
                szt = sz2[i]
                s.op("act", lambda e, PZ=PZ, szt=szt: e.activation(out=szt[:], in_=PZ[:, :], func=AF.Silu),
                     reads=pkz, writes=[("sz", i)])
                s.op("dve", lambda e, i=i, szt=szt: e.tensor_tensor(out=yy[i][:], in0=yb[i][:], in1=szt[:], op=ALU.mult),
                     reads=[("yb", i), ("sz", i)], writes=[("yy", i)])

                def tr(e, i=i):
                    ins = None
                    for kc in range(8):
                        ins = e.transpose(out=ptr[:, kc * 128:(kc + 1) * 128], in_=yy[i][:, kc * 128:(kc + 1) * 128],
                                          identity=ident[:])
                    return ins
                s.op("pe", tr, reads=[("yy", i), "ident"], writes=["ptr"])
                s.op("dve", lambda e, blk=blk: e.tensor_copy(out=yT[:, :, blk * 128:(blk + 1) * 128],
                                                             in_=ptr[:, :].rearrange("p (k t) -> p k t", k=8)),
                     reads=["ptr"], writes=[("yT", blk)])
                if pre:
                    pre.pop(0)()
            while pre:
                pre.pop(0)()
            if bi + 1 < len(branches):
                pre = load_w_chunks(Wz, 0, w_in, branches[bi + 1][1], 1024, "Wz", scale=gpre)
            else:
                pre = load_w_chunks(Wz, 0, w_o, 0, 1024, "Wz")
            it_ = 0

            def obank():
                i = st.get("ob_i", 0) % 7
                st["ob_i"] = st.get("ob_i", 0) + 1
                if i == 6:
                    return pn[:, :], [("pb", 6)]
                return pw[i // 2][:, (i % 2) * 512:(i % 2 + 1) * 512], [("pb", i)]
            for sbk in range(4):
                tsl = slice(sbk * 512, (sbk + 1) * 512)
                hkeys = [("hT", sbk * 4 + j) for j in range(4)]
                ykeys = [("yT", sbk * 4 + j) for j in range(4)]
                for mc in range(8):
                    P1, pk1 = obank()
                    mm_group(P1, [(Wout[:, kc, mc * 128:(mc + 1) * 128], yT[:, kc, tsl]) for kc in range(8)],
                             ykeys + ["Wout"], pk1)
                    P2, pk2 = obank()
                    mm_group(P2, [(Wgt[:, kc, mc * 128:(mc + 1) * 128], hT[:, kc, tsl]) for kc in range(8)],
                             hkeys + ["Wgt"], pk2)
                    s.op("act", lambda e, P2=P2: e.activation(out=sg[:], in_=P2, func=AF.Sigmoid),
                         reads=pk2, writes=["sg"])
                    mkey = ("mT", mc, sbk)
                    if bi == 0:
                        s.op("dve", lambda e, P1=P1, mc=mc, tsl=tsl: e.tensor_tensor(
                            out=mergedT[:, mc, tsl], in0=P1, in1=sg[:], op=ALU.mult),
                            reads=pk1 + ["sg"], writes=[mkey])
                    else:
                        s.op("dve", lambda e, P1=P1: e.tensor_tensor(out=tmpm[:], in0=P1, in1=sg[:], op=ALU.mult),
                             reads=pk1 + ["sg"], writes=["tmpm"])
                        s.op("dve", lambda e, mc=mc, tsl=tsl: e.tensor_tensor(
                            out=mergedT[:, mc, tsl], in0=mergedT[:, mc, tsl], in1=tmpm[:], op=ALU.add),
                            reads=["tmpm", mkey], writes=[mkey])
                    it_ += 1
                    if pre and it_ % 4 == 0:
                        pre.pop(0)()
            while pre:
                pre.pop(0)()
        tap("mergedT", mergedT[:, :, :], [128, 8, NOWN], [("mT", mc, sbk) for mc in range(8) for sbk in range(4)], BF16)
        Wo = Wz
        gpost = esb("gpost", [128, 1024])
        load_const("c1", gpost[:], g_post_d, "gpost")
        for blk in range(16):
            sbk = blk // 4
            PF, pkf = pwide()
            for half in range(2):
                mm_group(PF[:, half * 512:(half + 1) * 512],
                         [(mergedT[:, mc, blk * 128:(blk + 1) * 128], Wo[:, mc, half * 512:(half + 1) * 512]) for mc in range(8)],
                         [("mT", mc, sbk) for mc in range(8)] + ["Wz"], [pkf[half]])
            c = stat_cols()
            s.op("act", lambda e, PF=PF, c=c: e.activation(out=junk[:], in_=PF[:, :], func=AF.Square,
                                                           accum_out=stat[:, c:c + 1]),
                 reads=pkf, writes=["junk", ("stat", c)])
            rstd_from_ss(c, 1, D)
            i = st["x_i"] % 2
            st["x_i"] += 1
            dma("x%d" % i, xin[i][:], x_own[blk * 128:(blk + 1) * 128, :], [], [("xin", i)])
            r_ = wst[blk % 2][:, 0:1024]
            s.op("dve", lambda e, PF=PF, c=c, r_=r_: e.scalar_tensor_tensor(
                out=r_, in0=PF[:, :], scalar=stat[:, c:c + 1], in1=gpost[:], op0=ALU.mult, op1=ALU.mult),
                reads=pkf + [("stat", c), "gpost"], writes=[("wst", blk % 2)])
            s.op("dve", lambda e, r_=r_, i=i: e.tensor_tensor(out=r_, in0=r_, in1=xin[i][:], op=ALU.add),
                 reads=[("wst", blk % 2), ("xin", i)], writes=[("wst", blk % 2)])
            dma("res%d" % (blk % 2), out[blk * 128:(blk + 1) * 128, :], r_, [("wst", blk % 2)], [("out", blk)], eng="pool")
        el.close()
        s.barrier()

    global LAST_S
    LAST_S = s
    sem_names = list(ENGS) + sorted(s.dma_sems)
    sems = {n: es.enter_context(nc.semaphore(n)) for n in sem_names}
    needed = {e: set() for e in ENGS}
    for e in ENGS:
        for (waits, fn, sem, inc) in s.ops[e]:
            for (wn, wv) in waits:
                if wn in needed:
                    needed[wn].add(wv)
    rank = {e: {v: k + 1 for k, v in enumerate(sorted(needed[e]))} for e in ENGS}
    for e in ENGS:
        cnt = 0
        new_ops = []
        for (waits, fn, sem, inc) in s.ops[e]:
            w2 = [(wn, rank[wn][wv]) if wn in rank else (wn, wv) for (wn, wv) in waits]
            if sem in rank:
                cnt += 1
                do_inc = cnt in rank[sem]
                new_ops.append((w2, fn, sem, 1 if do_inc else 0))
            else:
                new_ops.append((w2, fn, sem, inc))
        s.ops[e] = new_ops
    final_waits = [(n, s.count[n]) for n in sorted(s.dma_sems)]
    with nc.Block() as block:
        def replay(eng_name):
            def body(e):
                for (waits, fn, sem, inc) in s.ops[eng_name]:
                    for (wn, wv) in waits:
                        e.wait_ge(sems[wn], wv)
                    ins = fn(e)
                    if inc:
                        ins.then_inc(sems[sem], inc)
                if eng_name == "sp":
                    for (wn, wv) in final_waits:
                        e.wait_ge(sems[wn], wv)
            return body

        block.tensor(replay("pe"))
        block.scalar(replay("act"))
        block.vector(replay("dve"))
        block.gpsimd(replay("pool"))
        block.sync(replay("sp"))
    es.close()
    return nc, tapd


def _bf16(a):
    return np.asarray(a, dtype=np.float32).astype(ml_dtypes.bfloat16)


def _t5_bucket(dist):
    d = np.maximum(dist, 1).astype(np.float32)
    large = 16 + (np.log(d / 16) / np.log(128 / 16) * 16).astype(np.int32)
    large = np.minimum(large, 31)
    return np.where(dist < 16, dist, large)


def _consts():
    i = np.arange(128)
    same = (i[:, None] // 64) == (i[None, :] // 64)
    tribd = (same & (i[:, None] <= i[None, :])).astype(np.float32)
    trirev = (same & (i[:, None] > i[None, :])).astype(np.float32)
    return {"ident": _bf16(np.eye(128)), "tribd": _bf16(tribd), "trirev": _bf16(trirev)}


def make_in_maps(inp):
    f = lambda a: np.ascontiguousarray(np.asarray(a, dtype=np.float32))
    cst = _consts()
    shared = {
        "w_in": f(inp["w_in"][0]),
        "w_gla_out": f(inp["w_gla_out"][0]),
        "w_o": f(inp["w_o"][0]),
        "w_mem_kv": f(inp["w_mem_kv"][0]),
        "w_dsa_out": f(inp["w_dsa_out"][0]),
        "w_x_out": f(inp["w_x_out"][0]),
        "g_mem": f(np.asarray(inp["g_mem"][0]).reshape(8, 128).T),
        "g_pre": f(np.asarray(inp["g_pre"][0]).reshape(8, 128).T),
        "g_post": f(np.broadcast_to(np.asarray(inp["g_post"][0])[None, :], (128, D))),
        "g_gla": f(np.broadcast_to(np.asarray(inp["g_gla"][0])[None, :], (128, 256))),
        "g_gla8": f(np.tile(np.asarray(inp["g_gla"][0]), 4).reshape(8, 128).T),
        "w_a_up": f(inp["w_gla_a_up"][0]),
        "b_a": f(np.asarray(inp["b_gla_a"][0])[None, :]),
    }
    shared.update(cst)
    rb = np.asarray(inp["rel_bias"], dtype=np.float32)
    sl = np.arange(128)[:, None]
    tl = np.arange(128)[None, :]
    bt = np.zeros((128, 2, 8, 128), np.float32)
    for dlt in range(2):
        dist = np.maximum(128 * dlt + tl - sl, 0)
        bt[:, dlt] = rb[_t5_bucket(dist)].transpose(0, 2, 1)
    shared["biasT"] = f(bt.reshape(128, -1))
    shared["cfarT"] = f(np.broadcast_to(rb[31][None, :, None], (128, 8, 128)).reshape(128, -1))
    shared["cmask"] = f(np.where(tl.T >= sl.T, 0.0, NEG))
    shared["bigI4"] = _bf16(np.tile(np.eye(128, dtype=np.float32) * 30000.0, (1, 4)))
    shared["pow2"] = f(np.broadcast_to((2.0 ** -np.arange(32))[None, :], (128, 32)))
    maps = []
    x = np.asarray(inp["x"], dtype=np.float32)
    for c in range(8):
        b, hf = c // 2, c % 2
        m = dict(shared)
        m["x_own"] = f(x[b, hf * NOWN:(hf + 1) * NOWN])
        m["mem"] = f(inp["mem"][b])
        m["ctxb"] = np.full((128, 1), 0.0 if hf == 1 else NEG, np.float32)
        m["x_ctx"] = f(x[b, 0:NCTX]) if hf == 1 else np.zeros((NCTX, D), np.float32)
        maps.append(m)
    return maps


def kernel(**inputs):
    nc, _ = build_program()
    maps = make_in_maps(inputs)
    res = run_bass_kernel_spmd(nc, maps, core_ids=list(range(8)))
    outp = np.zeros((4, NTOK, D), np.float32)
    for c in range(8):
        b, hf = c // 2, c % 2
        outp[b, hf * NOWN:(hf + 1) * NOWN] = np.asarray(res.results[c]["out"], dtype=np.float32)
    return outp
```
